# Optimizing a Trainium2 kernel written in Bass

```python
import math
import jax, jax.numpy as jnp
from jax import lax
import numpy as np

D_MODEL = 2048
BATCH = 8
SEQ = 2048
DEPTH = 1

MIX_WIDTH = D_MODEL
HG_DK = 128
HG_DV = 128
HG_HEADS = (MIX_WIDTH // 2) // HG_DV
HG_KEY = HG_HEADS * HG_DK
HG_VAL = HG_HEADS * HG_DV
HG_CHUNK = 64
SWA_HEAD_DIM = 64
SWA_HEADS = (MIX_WIDTH // 2) // SWA_HEAD_DIM
SWA_KV_HEADS = SWA_HEADS // 8
SWA_GROUP = SWA_HEADS // SWA_KV_HEADS
SWA_WIDTH = SWA_HEADS * SWA_HEAD_DIM
SWA_KV_WIDTH = SWA_KV_HEADS * SWA_HEAD_DIM
WINDOW = 128
IN_SPLITS = (HG_KEY, HG_KEY, HG_VAL, HG_VAL, SWA_WIDTH, SWA_KV_WIDTH, SWA_KV_WIDTH)
IN_COLS = sum(IN_SPLITS)
IN_OFFSETS = [int(o) for o in np.cumsum(IN_SPLITS)[:-1]]
ATTN_COLS = SWA_WIDTH + 2 * SWA_KV_WIDTH
N_EXPERTS = 32
TOP_K = 4
D_FF = D_MODEL
SWIGLU_LIMIT = 7.0
SWIGLU_ALPHA = 1.702
MOE_BLOCK = 128
DEEPNORM_ALPHA = (2.0 * DEPTH) ** 0.25
DEEPNORM_BETA = (8.0 * DEPTH) ** -0.25
NORM_EPS = 1e-5

kernel_name = "hgrn2_swa_sink_moe_deepnorm"


def layer_norm(x, w, b):
    xf = x.astype(jnp.float32)
    mu = jnp.mean(xf, axis=-1, keepdims=True)
    var = jnp.mean(jnp.square(xf - mu), axis=-1, keepdims=True)
    y = (xf - mu) * lax.rsqrt(var + NORM_EPS) * w.astype(jnp.float32) + b.astype(jnp.float32)
    return y.astype(x.dtype)


def rms_norm(x, w):
    xf = x.astype(jnp.float32)
    y = xf * lax.rsqrt(jnp.mean(xf * xf, axis=-1, keepdims=True) + NORM_EPS) * w.astype(jnp.float32)
    return y.astype(x.dtype)


def hgrn2_chunk_scan(q, k, v, log_f):
    bsz, seq, n_heads, dk = q.shape
    dv = v.shape[-1]
    nc = seq // HG_CHUNK

    def to_chunks(t):
        return t.reshape(bsz, nc, HG_CHUNK, n_heads, t.shape[-1]).transpose(1, 0, 3, 2, 4)

    causal = jnp.tril(jnp.ones((HG_CHUNK, HG_CHUNK), dtype=bool))

    def step(state, inp):
        qc, kc, vc, gc = inp
        b = jnp.cumsum(gc, axis=2)
        o_inter = jnp.einsum('bhtk,bhkv->bhtv', qc * jnp.exp(b), state)
        diff = b[:, :, :, None, :] - b[:, :, None, :, :]
        decay = jnp.exp(jnp.where(causal[:, :, None], diff, -jnp.inf))
        scores = jnp.einsum('bhtk,bhsk,bhtsk->bhts', qc, kc, decay)
        o_intra = jnp.einsum('bhts,bhsv->bhtv', scores, vc)
        b_last = b[:, :, -1:, :]
        new_state = jnp.exp(b_last[:, :, 0, :, None]) * state + jnp.einsum(
            'bhsk,bhsv->bhkv', kc * jnp.exp(b_last - b), vc)
        return new_state, o_inter + o_intra

    state0 = jnp.zeros((bsz, n_heads, dk, dv), jnp.float32)
    _, outs = lax.scan(step, state0, (to_chunks(q), to_chunks(k), to_chunks(v), to_chunks(log_f)))
    return outs.transpose(1, 0, 3, 2, 4).reshape(bsz, seq, n_heads, dv)


def hgrn2_mixer(hq, hf, hi, hg, lower_bound, norm_w):
    bsz, seq, _ = hq.shape
    f32 = jnp.float32
    q = jax.nn.silu(hq.astype(f32))
    forget = lower_bound + (1.0 - lower_bound) * jax.nn.sigmoid(hf.astype(f32))
    log_f = jnp.log(forget)
    k = 1.0 - forget
    v = hi.astype(f32)

    def heads(t, d):
        return t.reshape(bsz, seq, HG_HEADS, d)

    o = hgrn2_chunk_scan(heads(q, HG_DK), heads(k, HG_DK), heads(v, HG_DV), heads(log_f, HG_DK))
    o = rms_norm(o, norm_w) * jax.nn.silu(heads(hg.astype(f32), HG_DV))
    return o.reshape(bsz, seq, HG_VAL).astype(hq.dtype)


def swa_sink_mixer(aq, ak, av, sinks, norm_w):
    bsz, seq, _ = aq.shape
    nb = seq // WINDOW
    q = aq.reshape(bsz, nb, WINDOW, SWA_KV_HEADS, SWA_GROUP, SWA_HEAD_DIM)

    def band(t):
        t = t.reshape(bsz, seq, SWA_KV_HEADS, SWA_HEAD_DIM)
        t = jnp.concatenate([jnp.zeros_like(t[:, :WINDOW]), t], axis=1)
        t = t.reshape(bsz, nb + 1, WINDOW, SWA_KV_HEADS, SWA_HEAD_DIM)
        return jnp.concatenate([t[:, :-1], t[:, 1:]], axis=2)

    k = band(ak)
    v = band(av)
    scores = jnp.einsum('bnqhgd,bnkhd->bnhgqk', q, k).astype(jnp.float32) * (SWA_HEAD_DIM ** -0.5)
    q_pos = jnp.arange(nb)[:, None, None] * WINDOW + jnp.arange(WINDOW)[None, :, None]
    k_pos = jnp.arange(nb)[:, None, None] * WINDOW - WINDOW + jnp.arange(2 * WINDOW)[None, None, :]
    allowed = (k_pos <= q_pos) & (k_pos > q_pos - WINDOW) & (k_pos >= 0)
    scores = jnp.where(allowed[None, :, None, None], scores, -jnp.inf)
    sink = jnp.broadcast_to(
        sinks.astype(jnp.float32).reshape(1, 1, SWA_KV_HEADS, SWA_GROUP, 1, 1),
        scores.shape[:-1] + (1,))
    probs = jax.nn.softmax(jnp.concatenate([scores, sink], axis=-1), axis=-1)[..., :-1]
    o = jnp.einsum('bnhgqk,bnkhd->bnqhgd', probs.astype(av.dtype), v).reshape(bsz, seq, SWA_WIDTH)
    return rms_norm(o, norm_w)


def clamped_swiglu(g, u):
    g = jnp.minimum(g, SWIGLU_LIMIT)
    u = jnp.clip(u, -SWIGLU_LIMIT, SWIGLU_LIMIT)
    return g * jax.nn.sigmoid(SWIGLU_ALPHA * g) * (u + 1.0)


def moe_ffn(x2, w_router, b_router, w_gate, b_gate, w_up, b_up, w_down, b_down):
    n_tok, d = x2.shape
    logits = (x2 @ w_router + b_router).astype(jnp.float32)
    top_vals, top_idx = lax.top_k(logits, TOP_K)
    gates = jax.nn.softmax(top_vals, axis=-1)
    n_assign = n_tok * TOP_K
    eid = top_idx.reshape(n_assign).astype(jnp.int32)
    tok = jnp.repeat(jnp.arange(n_tok, dtype=jnp.int32), TOP_K)
    gw = gates.reshape(n_assign)
    order = jnp.argsort(eid)
    eid_s, tok_s, gw_s = eid[order], tok[order], gw[order]
    counts = jnp.zeros((N_EXPERTS,), jnp.int32).at[eid].add(1)
    starts = jnp.cumsum(counts) - counts
    padded = (counts + MOE_BLOCK - 1) // MOE_BLOCK * MOE_BLOCK
    pad_ends = jnp.cumsum(padded)
    pad_starts = pad_ends - padded
    dest = pad_starts[eid_s] + (jnp.arange(n_assign, dtype=jnp.int32) - starts[eid_s])
    n_blocks = -(-n_assign // MOE_BLOCK) + N_EXPERTS
    n_rows = n_blocks * MOE_BLOCK
    row_tok = jnp.full((n_rows,), n_tok, jnp.int32).at[dest].set(tok_s)
    row_gate = jnp.zeros((n_rows,), x2.dtype).at[dest].set(gw_s.astype(x2.dtype))
    block_start = jnp.arange(n_blocks, dtype=jnp.int32) * MOE_BLOCK
    block_expert = jnp.minimum(jnp.searchsorted(pad_ends, block_start, side='right'), N_EXPERTS - 1)
    x_pad = jnp.concatenate([x2, jnp.zeros((1, d), x2.dtype)], axis=0)
    xb = x_pad[row_tok].reshape(n_blocks, MOE_BLOCK, d)

    def expert_block(args):
        xblk, e = args
        hg = xblk @ w_gate[e] + b_gate[e]
        hu = xblk @ w_up[e] + b_up[e]
        return clamped_swiglu(hg, hu) @ w_down[e] + b_down[e]

    yb = lax.map(expert_block, (xb, block_expert)).reshape(n_rows, d)
    y = jax.ops.segment_sum(yb * row_gate[:, None], row_tok, num_segments=n_tok + 1)
    return y[:n_tok]


def setup_inputs(seed: int = 0) -> dict:
    key = jax.random.key(seed)
    ks = jax.random.split(key, 24)
    f32 = jnp.float32

    def nrm(k, shape, scale):
        return jax.random.normal(k, shape, f32) * scale

    return {
        "x": nrm(ks[0], (BATCH, SEQ, D_MODEL), 1.0),
        "ln_emb_w": 1.0 + nrm(ks[1], (D_MODEL,), 0.02),
        "ln_emb_b": nrm(ks[2], (D_MODEL,), 0.02),
        "w_in": nrm(ks[3], (DEPTH, D_MODEL, IN_COLS), D_MODEL ** -0.5),
        "b_qkv": nrm(ks[4], (DEPTH, ATTN_COLS), 0.02),
        "lb_logits": nrm(ks[5], (DEPTH + 1, HG_KEY), 0.5),
        "hg_norm_w": 1.0 + nrm(ks[6], (DEPTH, HG_DV), 0.02),
        "attn_sinks": nrm(ks[7], (DEPTH, SWA_HEADS), 0.5),
        "attn_norm_w": 1.0 + nrm(ks[8], (DEPTH, SWA_WIDTH), 0.02),
        "w_out": nrm(ks[9], (DEPTH, MIX_WIDTH, D_MODEL), MIX_WIDTH ** -0.5 * DEEPNORM_BETA),
        "b_out": nrm(ks[10], (DEPTH, D_MODEL), 0.02),
        "ln1_w": 1.0 + nrm(ks[11], (DEPTH, D_MODEL), 0.02),
        "ln1_b": nrm(ks[12], (DEPTH, D_MODEL), 0.02),
        "w_router": nrm(ks[13], (DEPTH, D_MODEL, N_EXPERTS), D_MODEL ** -0.5),
        "b_router": nrm(ks[14], (DEPTH, N_EXPERTS), 0.01),
        "w_gate": nrm(ks[15], (DEPTH, N_EXPERTS, D_MODEL, D_FF), D_MODEL ** -0.5),
        "b_gate": nrm(ks[16], (DEPTH, N_EXPERTS, D_FF), 0.02),
        "w_up": nrm(ks[17], (DEPTH, N_EXPERTS, D_MODEL, D_FF), D_MODEL ** -0.5),
        "b_up": nrm(ks[18], (DEPTH, N_EXPERTS, D_FF), 0.02),
        "w_down": nrm(ks[19], (DEPTH, N_EXPERTS, D_FF, D_MODEL), D_FF ** -0.5 * DEEPNORM_BETA),
        "b_down": nrm(ks[20], (DEPTH, N_EXPERTS, D_MODEL), 0.02),
        "ln2_w": 1.0 + nrm(ks[21], (DEPTH, D_MODEL), 0.02),
        "ln2_b": nrm(ks[22], (DEPTH, D_MODEL), 0.02),
    }


def reference(x, ln_emb_w, ln_emb_b, w_in, b_qkv, lb_logits, hg_norm_w, attn_sinks, attn_norm_w,
              w_out, b_out, ln1_w, ln1_b, w_router, b_router, w_gate, b_gate, w_up, b_up,
              w_down, b_down, ln2_w, ln2_b):
    bsz, seq, d = x.shape
    lower_bounds = jnp.cumsum(jax.nn.softmax(lb_logits.astype(jnp.float32), axis=0), axis=0)
    h = layer_norm(x, ln_emb_w, ln_emb_b)
    for l in range(DEPTH):
        proj = jnp.einsum('bsd,dc->bsc', h, w_in[l])
        hq, hf, hi, hg, aq, ak, av = jnp.split(proj, IN_OFFSETS, axis=-1)
        bq, bk, bv = jnp.split(b_qkv[l], [SWA_WIDTH, SWA_WIDTH + SWA_KV_WIDTH])
        y_hg = hgrn2_mixer(hq, hf, hi, hg, lower_bounds[l], hg_norm_w[l])
        y_sw = swa_sink_mixer(aq + bq, ak + bk, av + bv, attn_sinks[l], attn_norm_w[l])
        mix = jnp.concatenate([y_hg, y_sw.astype(h.dtype)], axis=-1) @ w_out[l] + b_out[l]
        h = layer_norm(DEEPNORM_ALPHA * h + mix, ln1_w[l], ln1_b[l])
        ffn = moe_ffn(h.reshape(bsz * seq, d), w_router[l], b_router[l], w_gate[l], b_gate[l],
                      w_up[l], b_up[l], w_down[l], b_down[l]).reshape(bsz, seq, d)
        h = layer_norm(DEEPNORM_ALPHA * h + ffn, ln2_w[l], ln2_b[l])
    return h
```

```python
import contextlib
import numpy as np
import concourse.bass as bass
import concourse.mybir as mybir
from concourse.bass_utils import run_bass_kernel_spmd

F32 = mybir.dt.float32
BF16 = mybir.dt.bfloat16
I32 = mybir.dt.int32
U32 = mybir.dt.uint32
AF = mybir.ActivationFunctionType
ALU = mybir.AluOpType
AX = mybir.AxisListType

D = 2048
KD = 16
ALPHA = 2.0 ** 0.25
EPS = 1e-5
NEG = -30000.0


class Buf:
    __slots__ = ("w", "r")

    def __init__(self):
        self.w = None
        self.r = {}


class Sched:
    ENGS = ("pe", "act", "dve", "pool", "sp")
    K = 8

    def __init__(self, nc, st, window=8):
        self.nc, self.st, self.window = nc, st, window
        self.prog = {e: [] for e in self.ENGS}
        self.sem, self.cnt, self.nsem = {}, {}, 0
        self.idx = {e: 0 for e in self.ENGS}
        self.seen = {e: {} for e in self.ENGS}
        self.dq = {}
        self.ndma = 0
        for e in ("pe", "act", "dve", "pool"):
            self._rot(e)

    def _newsem(self, nm):
        self.nsem += 1
        return self.st.enter_context(self.nc.semaphore(f"{nm}_{self.nsem}"))

    def _rot(self, e):
        self.sem[e] = self._newsem("s" + e)
        self.cnt[e] = 0

    def _deps(self, reads, writes):
        deps = []
        for b in reads:
            deps.append(b.w)
        for b in writes:
            deps.append(b.w)
            deps.extend(b.r.values())
        return deps

    def _waits(self, eng, deps):
        best = {}
        for t in deps:
            if t is None:
                continue
            sem, val, teng, tidx = t
            if teng == eng:
                if eng == "pe" or self.idx[eng] - tidx > self.window:
                    continue
            k = id(sem)
            if self.seen[eng].get(k, 0) >= val:
                continue
            if k not in best or best[k][1] < val:
                best[k] = (sem, val)
        out = []
        for k, (sem, val) in best.items():
            self.seen[eng][k] = val
            out.append((sem, val))
        return out

    def op(self, eng, meth, reads=(), writes=(), **kw):
        waits = self._waits(eng, self._deps(reads, writes))
        self.cnt[eng] += 1
        self.idx[eng] += 1
        tk = (self.sem[eng], self.cnt[eng], eng, self.idx[eng])
        self.prog[eng].append((waits, meth, kw, self.sem[eng], 1))
        if self.cnt[eng] >= 30000:
            self._rot(eng)
        for b in reads:
            b.r[eng] = tk
        for b in writes:
            b.w = tk
            b.r = {}
        return tk

    def dma(self, q, reads=(), writes=(), meth="dma_start", **kw):
        d = self.dq.get(q)
        if d is None:
            d = self.dq[q] = {"sems": [self._newsem("d" + q) for _ in range(self.K)], "cnt": [0] * self.K, "n": 0}
        i = d["n"] % self.K
        d["n"] += 1
        self.ndma += 1
        sem, prev = d["sems"][i], d["cnt"][i]
        deps = self._deps(reads, writes)
        if prev > 0:
            deps.append((sem, prev, None, 0))
        waits = self._waits(q, deps)
        d["cnt"][i] = prev + 16
        tk = (sem, prev + 16, None, 0)
        self.prog[q].append((waits, meth, kw, sem, 16))
        for b in reads:
            b.r[("dma", self.ndma)] = tk
        for b in writes:
            b.w = tk
            b.r = {}
        return tk

    def special(self, q, buf, meth, **kw):
        sem = self._newsem("x" + q)
        self.prog[q].append(([], meth, kw, sem, 16))
        buf.w = (sem, 16, None, 0)
        buf.r = {}

    def barrier(self):
        last = []
        for e in ("pe", "act", "dve", "pool"):
            if self.cnt[e] > 0:
                last.append((self.sem[e], self.cnt[e], e, self.idx[e]))
        for q, d in self.dq.items():
            for sem, c in zip(d["sems"], d["cnt"]):
                if c > 0:
                    last.append((sem, c, None, 0))
        for e in self.ENGS:
            deps = [t for t in last if t[2] != e]
            waits = self._waits(e, deps)
            if waits:
                self.prog[e].append((waits, None, None, None, 0))

    def wait_all(self, eng, tickets):
        waits = self._waits(eng, list(tickets))
        self.prog[eng].append((waits, None, None, None, 0))

    def emit(self, block):
        m = {"pe": block.tensor, "act": block.scalar, "dve": block.vector, "pool": block.gpsimd, "sp": block.sync}
        for e in self.ENGS:
            prog = self.prog[e]

            def body(eng, prog=prog):
                for waits, meth, kw, sem, inc in prog:
                    for s, v in waits:
                        eng.wait_ge(s, v)
                    if meth is not None:
                        try:
                            getattr(eng, meth)(**kw).then_inc(sem, inc)
                        except Exception:
                            print("FAILED OP", meth, {k: str(v)[:400] for k, v in kw.items()})
                            if meth == "indirect_dma_start":
                                print("IDX AP", kw["out_offset"].ap if kw.get("out_offset") is not None else None)
                                for drop in (("bounds_check", "oob_is_err"),):
                                    kw2 = {k: v for k, v in kw.items() if k not in drop}
                                    try:
                                        getattr(eng, meth)(**kw2)
                                        print("WORKS without", drop)
                                    except Exception as ex2:
                                        print("still fails without", drop, repr(ex2)[:200])
                            raise

            m[e](body)


def build(cfg):
    NCORES, E, ELOC, DFF, C, SEQ = cfg["NCORES"], cfg["E"], cfg["ELOC"], cfg["DFF"], cfg["C"], cfg["SEQ"]
    STOP = cfg.get("STOP", "full")
    NT = SEQ // 128
    KF = DFF // 128
    CT = C // 128
    TB = 512
    NTB = SEQ // TB
    nc = bass.Bass("TRN2", target_bir_lowering=False)

    def din(name, shape, dt=F32):
        return nc.dram_tensor(name, shape, dt, kind="ExternalInput").ap()

    def dscr(name, shape, dt):
        return nc.dram_tensor(name, shape, dt, kind="Internal").ap()

    x = din("x", [SEQ, D])
    ln_emb_w, ln_emb_b = din("ln_emb_w", [1, D]), din("ln_emb_b", [1, D])
    w_in = din("w_in", [D, 5376])
    b_qkv = din("b_qkv", [1, 1280])
    lb_logits = din("lb_logits", [2, 1024])
    hg_norm_w = din("hg_norm_w", [1, 128])
    attn_sinks = din("attn_sinks", [1, 16])
    attn_norm_w = din("attn_norm_w", [1, 1024])
    w_out = din("w_out", [D, D])
    b_out = din("b_out", [1, D])
    ln1_w, ln1_b = din("ln1_w", [1, D]), din("ln1_b", [1, D])
    w_router = din("w_router", [D, E])
    b_router = din("b_router", [1, E])
    NP = cfg.get("NP", 1)
    EP = E // NP
    w_gate = [din(f"w_gate{p}", [EP * D, DFF]) for p in range(NP)]
    w_up = [din(f"w_up{p}", [EP * D, DFF]) for p in range(NP)]
    w_down = [din(f"w_down{p}", [EP * DFF, D]) for p in range(NP)]
    b_gate = din("b_gate", [E * KF, 128])
    b_up = din("b_up", [E * KF, 128])
    b_down = din("b_down", [E, D])
    ln2_w, ln2_b = din("ln2_w", [1, D]), din("ln2_b", [1, D])
    out = nc.dram_tensor("out", [SEQ, D], F32, kind="ExternalOutput").ap()
    dbg = None
    if STOP != "full":
        dbg = nc.dram_tensor("dbg", [SEQ, D], F32, kind="ExternalOutput").ap()

    h0_d = dscr("h0_d", [SEQ, D], F32)
    mix_d = dscr("mix_d", [SEQ, D], BF16)
    h1_d = dscr("h1_d", [SEQ, D], F32)
    h1b_d = dscr("h1b_d", [SEQ + 128, D], BF16)
    slot_d = dscr("slot_d", [E * C + 128, 16], I32)
    yall_d = dscr("yall_d", [E * C + 128, D], F32)
    with contextlib.ExitStack() as st:
        S = Sched(nc, st)

        def sb(name, shape, dt):
            return st.enter_context(nc.sbuf_tensor(name, shape, dt))

        ARENA_W = 43008
        arena = sb("arena", [128, ARENA_W], F32)
        atop = [0]

        def al(name, shape, dt):
            esz = 4 if dt in (F32, I32, U32) else 2
            n = 1
            for d_ in shape[1:]:
                n *= d_
            words = (n * esz + 3) // 4
            words = (words + 7) // 8 * 8
            a0 = atop[0]
            atop[0] += words
            assert atop[0] <= ARENA_W, (name, atop[0])
            v = arena[:, a0:a0 + words]
            if dt != F32:
                v = v.bitcast(dt)
            v = v[:, 0:n]
            if len(shape) == 3:
                v = v.rearrange("p (a b) -> p a b", a=shape[1])
            return v

        def release(mark):
            S.barrier()
            atop[0] = mark

        PS = [st.enter_context(nc.psum_tensor(f"ps{i}", [128, 512], F32)) for i in range(8)]
        PSB = [Buf() for _ in range(8)]
        psn = [0]

        def ps_get(n=1):
            if (psn[0] % 8) + n > 8:
                psn[0] += 8 - (psn[0] % 8)
            i = psn[0] % 8
            psn[0] += n
            return list(range(i, i + n))

        ident_f = sb("ident_f", [128, 128], F32)
        ident_b = sb("ident_b", [128, 128], BF16)
        ones_b = sb("ones_b", [128, 128], BF16)
        lstrict = sb("lstrict", [128, 128], BF16)
        hmask = sb("hmask", [128, 128], F32)
        amask = sb("amask", [128, 256], F32)
        m01 = sb("m01", [128, TB], F32)
        iota_e = sb("iota_e", [128, E], F32)
        rowbase = sb("rowbase", [128, E], F32)
        tokid = sb("tokid", [128, NT * 16], I32)
        padidx = sb("padidx", [128, 1], I32)
        zer_b = sb("zer_b", [128, D], BF16)
        CB = Buf()
        P = lambda meth, **kw: S.op("pool", meth, (), (CB,), **kw)
        P("memset", ap=ident_f[:], constant=1.0)
        P("affine_select", out=ident_f[:], in_=ident_f[:], pattern=[[-1, 128]], compare_op=ALU.is_equal, fill=0.0, base=0, channel_multiplier=1)
        P("tensor_copy", out=ident_b[:], in_=ident_f[:])
        P("memset", ap=ones_b[:], constant=1.0)
        P("memset", ap=hmask[:], constant=1.0)
        P("affine_select", out=hmask[:], in_=hmask[:], pattern=[[1, 128]], compare_op=ALU.is_ge, fill=0.0, base=-1, channel_multiplier=-1)
        P("tensor_copy", out=lstrict[:], in_=hmask[:])
        P("memset", ap=hmask[:], constant=1.0)
        P("affine_select", out=hmask[:], in_=hmask[:], pattern=[[1, 128]], compare_op=ALU.is_ge, fill=0.0, base=0, channel_multiplier=-1)
        P("memset", ap=hmask[0:64, 64:128], constant=0.0)
        P("memset", ap=amask[:], constant=0.0)
        P("affine_select", out=amask[:], in_=amask[:], pattern=[[1, 256]], compare_op=ALU.is_ge, fill=NEG, base=-1, channel_multiplier=-1)
        P("affine_select", out=amask[:], in_=amask[:], pattern=[[-1, 256]], compare_op=ALU.is_ge, fill=NEG, base=128, channel_multiplier=1)
        P("memset", ap=m01[:], constant=1.0)
        P("memset", ap=m01[:].rearrange("p (c t) -> p c t", t=64)[:, :, 0:1], constant=0.0)
        P("iota", out=iota_e[:], pattern=[[1, E]], base=0, channel_multiplier=0, allow_small_or_imprecise_dtypes=True)
        P("iota", out=rowbase[:], pattern=[[C, E]], base=0, channel_multiplier=0, allow_small_or_imprecise_dtypes=True)
        P("iota", out=tokid[:], pattern=[[128, NT], [0, 16]], base=0, channel_multiplier=1)
        P("memset", ap=padidx[:], constant=SEQ)
        P("memset", ap=zer_b[:], constant=0.0)

        pw = sb("pw", [128, D], F32)
        pb = sb("pb", [128, D], F32)
        PWB, PBB = Buf(), Buf()

        def load_bcast(dst, dbuf, src_row, n):
            S.dma("sp", (), (dbuf,), out=dst[:, 0:n], in_=src_row.partition_broadcast(128))

        stats = sb("stats", [128, 4, 6], F32)
        mv = sb("mv", [128, 2], F32)
        sm1 = sb("sm1", [128, 8], F32)
        STB = Buf()

        def layer_norm(src, sbuf, dst, dbuf, w_t, w_b, b_t, b_b):
            for c in range(4):
                S.op("dve", "bn_stats", (sbuf,), (STB,), out=stats[:, c, :], in_=src[:, c * 512:(c + 1) * 512])
            S.op("dve", "bn_aggr", (STB,), (STB,), out=mv[:], in_=stats[:].rearrange("p a b -> p (a b)"))
            S.op("dve", "tensor_scalar", (STB,), (STB,), out=sm1[:, 0:1], in0=mv[:, 1:2], scalar1=EPS, scalar2=None, op0=ALU.add)
            S.op("act", "activation", (STB,), (STB,), out=sm1[:, 1:2], in_=sm1[:, 0:1], func=AF.Sqrt)
            S.op("dve", "reciprocal", (STB,), (STB,), out=sm1[:, 2:3], in_=sm1[:, 1:2])
            S.op("dve", "scalar_tensor_tensor", (STB,), (STB,), out=sm1[:, 3:4], in0=mv[:, 0:1], scalar=-1.0, in1=sm1[:, 2:3], op0=ALU.mult, op1=ALU.mult)
            S.op("act", "activation", (sbuf, STB), (dbuf,), out=dst[:], in_=src[:], func=AF.Identity, bias=sm1[:, 3:4], scale=sm1[:, 2:3])
            S.op("dve", "tensor_tensor", (dbuf, w_b), (dbuf,), out=dst[:], in0=dst[:], in1=w_t[:], op=ALU.mult)
            S.op("pool", "tensor_tensor", (dbuf, b_b), (dbuf,), out=dst[:], in0=dst[:], in1=b_t[:], op=ALU.add)

        hT = al("hT", [128, KD, SEQ], BF16)
        mark_hT = atop[0]
        HTB = [Buf() for _ in range(NT)]
        tA = [al(f"tA{i}", [128, D], F32) for i in range(2)]
        tAB = [Buf(), Buf()]
        tB = [al(f"tB{i}", [128, D], F32) for i in range(2)]
        tBB = [Buf(), Buf()]
        tC = [al(f"tC{i}", [128, D], BF16) for i in range(2)]
        tCB = [Buf(), Buf()]

        out_tk = []

        WINB, WOUTB = Buf(), Buf()

        load_bcast(pw, PWB, ln_emb_w[0:1, :], D)
        load_bcast(pb, PBB, ln_emb_b[0:1, :], D)
        H0B = [Buf() for _ in range(NT)]
        for i in range(NT):
            a, ab = tA[i % 2], tAB[i % 2]
            b, bb = tB[i % 2], tBB[i % 2]
            c, cb = tC[i % 2], tCB[i % 2]
            S.dma("sp", (), (ab,), out=a[:], in_=x[i * 128:(i + 1) * 128, :])
            layer_norm(a, ab, b, bb, pw, PWB, pb, PBB)
            S.dma("sp", (bb,), (H0B[i],), out=h0_d[i * 128:(i + 1) * 128, :], in_=b[:])
            S.op("act", "activation", (bb,), (cb,), out=c[:], in_=b[:], func=AF.Copy)
            for g in range(4):
                (pi,) = ps_get()
                pv = PS[pi][:].bitcast(BF16)
                for j in range(4):
                    kd = g * 4 + j
                    S.op("pe", "transpose", (cb,), (PSB[pi],), out=pv[:, j * 128:(j + 1) * 128], in_=c[:, kd * 128:(kd + 1) * 128], identity=ident_b[:])
                S.op("dve" if g % 2 == 0 else "act", "tensor_copy" if g % 2 == 0 else "copy", (PSB[pi],), (HTB[i],),
                     out=hT[:, g * 4:(g + 1) * 4, i * 128:(i + 1) * 128],
                     in_=pv[:, 0:512].rearrange("p (j t) -> p j t", j=4))
        if STOP == "ln":
            for i in range(NT):
                out_tk.append(S.dma("sp", (H0B[i],), (), out=dbg[i * 128:(i + 1) * 128, :], in_=h0_d[i * 128:(i + 1) * 128, :]))

        release(mark_hT)
        lbt = sb("lbt", [128, 8, 4], F32)
        LBB = Buf()
        if STOP not in ("ln",):
            S.dma("sp", (), (LBB,), out=lbt[:, :, 0:1], in_=lb_logits[0:1, :].rearrange("o (h p) -> p h o", p=128), allow_slow_non_contiguous=True)
            S.dma("sp", (), (LBB,), out=lbt[:, :, 1:2], in_=lb_logits[1:2, :].rearrange("o (h p) -> p h o", p=128), allow_slow_non_contiguous=True)
            S.op("dve", "tensor_tensor", (LBB,), (LBB,), out=lbt[:, :, 2:3], in0=lbt[:, :, 0:1], in1=lbt[:, :, 1:2], op=ALU.subtract)
            S.op("act", "activation", (LBB,), (LBB,), out=lbt[:, :, 2:3], in_=lbt[:, :, 2:3], func=AF.Sigmoid)
            S.op("dve", "tensor_scalar", (LBB,), (LBB,), out=lbt[:, :, 3:4], in0=lbt[:, :, 2:3], scalar1=-1.0, scalar2=1.0, op0=ALU.mult, op1=ALU.add)
            hgw = sb("hgw", [128, 128], F32)
            HGWB = Buf()
            S.dma("sp", (), (HGWB,), out=hgw[:], in_=hg_norm_w[0:1, :].partition_broadcast(128))

            wh = [al(f"wh{i}", [128, KD, 512], BF16) for i in range(2)]
            WHB = [Buf(), Buf()]
            QT = al("QT", [128, SEQ], BF16)
            KT = al("KT", [128, SEQ], BF16)
            QTB, KTB = Buf(), Buf()
            QTa = al("QTa", [128, SEQ], BF16)
            QTb = al("QTb", [128, SEQ], BF16)
            QTaB, QTbB = Buf(), Buf()
            Vt0 = al("Vt0", [128, NT, 128], BF16)
            Vt1 = al("Vt1", [128, NT, 128], BF16)
            Vt0B, Vt1B = Buf(), Buf()
            S.op("pool", "memset", (), (QTaB,), ap=QTa[:], constant=0.0)
            S.op("pool", "memset", (), (QTbB,), ap=QTb[:], constant=0.0)
            S.op("pool", "memset", (), (Vt0B,), ap=Vt0[:], constant=0.0)
            S.op("pool", "memset", (), (Vt1B,), ap=Vt1[:], constant=0.0)
            EBL = al("EBL", [128, SEQ // 64], F32)
            EBLB = Buf()
            Ktm = al("Ktm", [128, NT, 128], BF16)
            Vtm = al("Vtm", [128, NT, 128], BF16)
            GW = al("GW", [128, NT, 128], F32)
            Yh = al("Yh", [128, NT, 128], BF16)
            KtmB, VtmB, GWB, YhB = Buf(), Buf(), Buf(), Buf()
            ft = [al(f"ft{i}", [128, TB], F32) for i in range(6)]
            FTB = [Buf() for _ in range(6)]
            Sf = [al(f"Sf{i}", [128, 128], F32) for i in range(2)]
            Sb_ = [al(f"Sb{i}", [128, 128], BF16) for i in range(2)]
            SfB = [Buf(), Buf()]
            SbB = [Buf(), Buf()]
            sT = [al(f"sT{i}", [128, 128], BF16) for i in range(2)]
            sTB = [Buf(), Buf()]
            tmpS = al("tmpS", [128, 128], F32)
            tmpSB = Buf()
            hsm = sb("hsm", [128, 8], F32)
            HSMB = Buf()
            junk = al("junk", [128, 128], F32)
            JB = Buf()

            NH = cfg.get("NH", 8)
            for hh in range(NH):
                w, wb = wh[hh % 2], WHB[hh % 2]
                for gi, base in enumerate((0, 1024, 2048, 3072)):
                    S.dma("pool", (WINB,), (wb,), out=w[:, :, gi * 128:(gi + 1) * 128],
                          in_=w_in[:, base + hh * 128: base + (hh + 1) * 128].rearrange("(k p) c -> p k c", p=128))
                for tb in range(NTB):
                    tok = slice(tb * TB, (tb + 1) * TB)
                    pq, pf = ps_get(2)
                    for kd in range(KD):
                        S.op("pe", "matmul", (wb,) + tuple(HTB[tb * 4:(tb + 1) * 4]), (PSB[pq],), out=PS[pq][:], lhsT=w[:, kd, 0:128], rhs=hT[:, kd, tok], start=(kd == 0), stop=(kd == KD - 1))
                    for kd in range(KD):
                        S.op("pe", "matmul", (wb,) + tuple(HTB[tb * 4:(tb + 1) * 4]), (PSB[pf],), out=PS[pf][:], lhsT=w[:, kd, 128:256], rhs=hT[:, kd, tok], start=(kd == 0), stop=(kd == KD - 1))
                    sig, f, lf, bb_, eb, enb = ft
                    S.op("act", "activation", (PSB[pf],), (FTB[0],), out=sig[:], in_=PS[pf][:], func=AF.Sigmoid)
                    S.op("dve", "tensor_scalar", (FTB[0], LBB), (FTB[1],), out=f[:], in0=sig[:], scalar1=lbt[:, hh, 3:4], scalar2=lbt[:, hh, 2:3], op0=ALU.mult, op1=ALU.add)
                    S.op("act", "activation", (FTB[1],), (FTB[2],), out=lf[:], in_=f[:], func=AF.Ln)
                    S.op("dve", "tensor_tensor_scan", (FTB[2], CB), (FTB[3],), out=bb_[:], data0=m01[:], data1=lf[:], initial=0.0, op0=ALU.mult, op1=ALU.add)
                    S.op("act", "activation", (FTB[3],), (FTB[4],), out=eb[:], in_=bb_[:], func=AF.Exp)
                    S.op("act", "activation", (FTB[3],), (FTB[5],), out=enb[:], in_=bb_[:], func=AF.Exp, scale=-1.0)
                    S.op("pool", "tensor_copy", (FTB[4],), (EBLB,), out=EBL[:, tb * 8:(tb + 1) * 8], in_=eb[:].rearrange("p (c t) -> p c t", t=64)[:, :, 63])
                    S.op("act", "activation", (PSB[pq],), (FTB[0],), out=sig[:], in_=PS[pq][:], func=AF.Sigmoid)
                    S.op("dve", "tensor_tensor", (FTB[0], PSB[pq]), (FTB[0],), out=sig[:], in0=sig[:], in1=PS[pq][:], op=ALU.mult)
                    S.op("dve", "tensor_tensor", (FTB[0], FTB[4]), (QTB,), out=QT[:, tok], in0=sig[:], in1=eb[:], op=ALU.mult)
                    qv = lambda t_: t_[:, tok].rearrange("p (c two t) -> p c two t", two=2, t=64)
                    S.op("pool", "tensor_copy", (QTB,), (QTaB,), out=qv(QTa)[:, :, 0, :], in_=qv(QT)[:, :, 0, :])
                    S.op("pool", "tensor_copy", (QTB,), (QTbB,), out=qv(QTb)[:, :, 1, :], in_=qv(QT)[:, :, 1, :])
                    S.op("pool", "tensor_scalar", (FTB[1],), (FTB[2],), out=lf[:], in0=f[:], scalar1=-1.0, scalar2=1.0, op0=ALU.mult, op1=ALU.add)
                    S.op("pool", "tensor_tensor", (FTB[2], FTB[5]), (KTB,), out=KT[:, tok], in0=lf[:], in1=enb[:], op=ALU.mult)
                HS = cfg.get("HSTOP", 3)
                for i in range(NT if HS >= 2 else 0):
                    (pv_,) = ps_get()
                    for kd in range(KD):
                        S.op("pe", "matmul", (wb, HTB[i]), (PSB[pv_],), out=PS[pv_][:, 0:256], lhsT=hT[:, kd, i * 128:(i + 1) * 128], rhs=w[:, kd, 256:512], start=(kd == 0), stop=(kd == KD - 1))
                    S.op("act", "copy", (PSB[pv_],), (VtmB,), out=Vtm[:, i, :], in_=PS[pv_][:, 0:128])
                    S.op("act", "copy", (PSB[pv_],), (Vt0B,), out=Vt0[0:64, i, :], in_=PS[pv_][0:64, 0:128])
                    S.op("act", "copy", (PSB[pv_],), (Vt1B,), out=Vt1[64:128, i, :], in_=PS[pv_][64:128, 0:128])
                    S.op("act", "activation", (PSB[pv_],), (JB,), out=junk[:], in_=PS[pv_][:, 128:256], func=AF.Sigmoid)
                    S.op("dve", "tensor_tensor", (JB, PSB[pv_]), (JB,), out=junk[:], in0=junk[:], in1=PS[pv_][:, 128:256], op=ALU.mult)
                    S.op("pool", "tensor_tensor", (JB, HGWB), (GWB,), out=GW[:, i, :], in0=junk[:], in1=hgw[:], op=ALU.mult)
                    (pt,) = ps_get()
                    ptv = PS[pt][:].bitcast(BF16)
                    S.op("pe", "transpose", (KTB,), (PSB[pt],), out=ptv[:, 0:128], in_=KT[:, i * 128:(i + 1) * 128], identity=ident_b[:])
                    S.op("dve", "tensor_copy", (PSB[pt],), (KtmB,), out=Ktm[:, i, :], in_=ptv[:, 0:128])
                S.op("pool", "memset", (), (SfB[0],), ap=Sf[0][:], constant=0.0)
                S.op("pool", "memset", (), (SbB[0],), ap=Sb_[0][:], constant=0.0)
                cur = 0
                for i in range(NT if HS >= 3 else 0):
                    tk_ = slice(i * 128, (i + 1) * 128)
                    (psc,) = ps_get()
                    S.op("pe", "matmul", (KTB, QTB), (PSB[psc],), out=PS[psc][:, 0:128], lhsT=KT[:, tk_], rhs=QT[:, tk_], start=True, stop=True)
                    s_, s_b = sT[i % 2], sTB[i % 2]
                    S.op("dve", "tensor_tensor", (PSB[psc], CB), (s_b,), out=s_[:], in0=PS[psc][:, 0:128], in1=hmask[:], op=ALU.mult)
                    (po,) = ps_get()
                    S.op("pe", "matmul", (s_b, VtmB), (PSB[po],), out=PS[po][:, 0:128], lhsT=s_[:], rhs=Vtm[:, i, :], start=True, stop=False)
                    for c in range(2):
                        rows = slice(c * 64, (c + 1) * 64)
                        S.op("pe", "matmul", (QTaB, QTbB, SbB[cur]), (PSB[po],), out=PS[po][:, 0:128], lhsT=(QTa, QTb)[c][:, tk_], rhs=Sb_[cur][:], start=False, stop=(c == 1))
                        (pp,) = ps_get()
                        S.op("pe", "matmul", (KtmB, Vt0B, Vt1B), (PSB[pp],), out=PS[pp][:, 0:128], lhsT=Ktm[:, i, :], rhs=(Vt0, Vt1)[c][:, i, :], start=True, stop=True)
                        nxt = 1 - cur
                        S.op("dve", "tensor_tensor", (PSB[pp], SfB[cur]), (tmpSB,), out=tmpS[:], in0=PS[pp][:, 0:128], in1=Sf[cur][:], op=ALU.add)
                        S.op("dve", "tensor_scalar", (tmpSB, EBLB), (SfB[nxt],), out=Sf[nxt][:], in0=tmpS[:], scalar1=EBL[:, 2 * i + c: 2 * i + c + 1], scalar2=None, op0=ALU.mult)
                        S.op("act", "copy", (SfB[nxt],), (SbB[nxt],), out=Sb_[nxt][:], in_=Sf[nxt][:])
                        cur = nxt
                    S.op("act", "activation", (PSB[po],), (JB, HSMB), out=junk[:], in_=PS[po][:, 0:128], func=AF.Square, accum_out=hsm[:, 0:1])
                    S.op("dve", "tensor_scalar", (HSMB,), (HSMB,), out=hsm[:, 1:2], in0=hsm[:, 0:1], scalar1=1.0 / 128, scalar2=EPS, op0=ALU.mult, op1=ALU.add)
                    S.op("act", "activation", (HSMB,), (HSMB,), out=hsm[:, 2:3], in_=hsm[:, 1:2], func=AF.Sqrt)
                    S.op("dve", "reciprocal", (HSMB,), (HSMB,), out=hsm[:, 3:4], in_=hsm[:, 2:3])
                    S.op("dve", "scalar_tensor_tensor", (PSB[po], HSMB, GWB), (YhB,), out=Yh[:, i, :], in0=PS[po][:, 0:128], scalar=hsm[:, 3:4], in1=GW[:, i, :], op0=ALU.mult, op1=ALU.mult)
                S.dma("sp", (YhB,), (), out=mix_d[:, hh * 128:(hh + 1) * 128].rearrange("(i p) v -> p i v", p=128), in_=Yh[:])
            if STOP == "hgrn":
                S.op("pool", "tensor_copy", (YhB,), (tAB[0],), out=tA[0][:, 0:NT * 128].rearrange("p (i v) -> p i v", v=128), in_=Yh[:])
                out_tk.append(S.dma("sp", (tAB[0],), (), out=dbg[0:128, 0:NT * 128], in_=tA[0][:, 0:NT * 128]))

        IOA = bass.IndirectOffsetOnAxis
        if STOP not in ("ln", "hgrn"):
            release(mark_hT)
            wq = [al(f"wq{i}", [128, KD, 128], BF16) for i in range(2)]
            WQB = [Buf(), Buf()]
            wkv = al("wkv", [128, KD, 256], BF16)
            WKVB = Buf()
            qT = al("qT", [128, 8, SEQ], BF16)
            QB = [Buf() for _ in range(8)]
            kTa = al("kTa", [128, SEQ], BF16)
            kTb = al("kTb", [128, SEQ], BF16)
            KAB, KBB = Buf(), Buf()
            Vs = al("Vs", [128, NT, 128], BF16)
            VSB = Buf()
            bqt = al("bqt", [128, 8, 1], F32)
            bkt = al("bkt", [128, 1, 1], F32)
            bvb = al("bvb", [128, 128], F32)
            sink_t = al("sink_t", [128, 16], F32)
            anw = al("anw", [128, 1024], F32)
            PRB = Buf()
            S.dma("sp", (), (PRB,), out=bqt[0:64, :, :], in_=b_qkv[0:1, 0:512].rearrange("o (j p) -> p j o", p=64), allow_slow_non_contiguous=True)
            S.dma("sp", (), (PRB,), out=bqt[64:128, :, :], in_=b_qkv[0:1, 512:1024].rearrange("o (j p) -> p j o", p=64), allow_slow_non_contiguous=True)
            S.dma("sp", (), (PRB,), out=bkt[:, :, :], in_=b_qkv[0:1, 1024:1152].rearrange("o (j p) -> p j o", p=128), allow_slow_non_contiguous=True)
            S.dma("sp", (), (PRB,), out=bvb[:, :], in_=b_qkv[0:1, 1152:1280].partition_broadcast(128))
            S.dma("sp", (), (PRB,), out=sink_t[:, :], in_=attn_sinks[0:1, :].partition_broadcast(128))
            S.dma("sp", (), (PRB,), out=anw[:, :], in_=attn_norm_w[0:1, :].partition_broadcast(128))
            S.op("dve", "tensor_scalar", (PRB,), (PRB,), out=bqt[:, :, 0], in0=bqt[:, :, 0], scalar1=0.125, scalar2=None, op0=ALU.mult)
            S.op("pool", "memset", (), (KAB,), ap=kTa[:], constant=0.0)
            S.op("pool", "memset", (), (KBB,), ap=kTb[:], constant=0.0)
            for half, c0 in ((0, 5120), (1, 5248)):
                S.dma("pool", (WINB,), (WKVB,), out=wkv[:, :, half * 128:(half + 1) * 128], in_=w_in[:, c0:c0 + 128].rearrange("(k p) c -> p k c", p=128))
            allh = tuple(HTB)
            for tb in range(NTB):
                tok = slice(tb * TB, (tb + 1) * TB)
                (pk,) = ps_get()
                for kd in range(KD):
                    S.op("pe", "matmul", (WKVB,) + allh, (PSB[pk],), out=PS[pk][:], lhsT=wkv[:, kd, 0:128], rhs=hT[:, kd, tok], start=(kd == 0), stop=(kd == KD - 1))
                S.op("act", "activation", (PSB[pk], PRB), (KAB,), out=kTa[0:64, tok], in_=PS[pk][0:64, :], func=AF.Identity, bias=bkt[0:64, 0, :], scale=1.0)
                S.op("act", "activation", (PSB[pk], PRB), (KBB,), out=kTb[64:128, tok], in_=PS[pk][64:128, :], func=AF.Identity, bias=bkt[64:128, 0, :], scale=1.0)
            for i in range(NT):
                (pv_,) = ps_get()
                for kd in range(KD):
                    S.op("pe", "matmul", (WKVB,) + allh, (PSB[pv_],), out=PS[pv_][:, 0:128], lhsT=hT[:, kd, i * 128:(i + 1) * 128], rhs=wkv[:, kd, 128:256], start=(kd == 0), stop=(kd == KD - 1))
                S.op("dve", "tensor_tensor", (PSB[pv_], PRB), (VSB,), out=Vs[:, i, :], in0=PS[pv_][:, 0:128], in1=bvb[:], op=ALU.add)
            for j in range(8):
                w, wb = wq[j % 2], WQB[j % 2]
                for half, hd in ((0, j), (1, 8 + j)):
                    S.dma("pool", (WINB,), (wb,), out=w[:, :, half * 64:(half + 1) * 64], in_=w_in[:, 4096 + hd * 64: 4096 + (hd + 1) * 64].rearrange("(k p) c -> p k c", p=128))
                for tb in range(NTB):
                    tok = slice(tb * TB, (tb + 1) * TB)
                    (pq,) = ps_get()
                    for kd in range(KD):
                        S.op("pe", "matmul", (wb,) + allh, (PSB[pq],), out=PS[pq][:], lhsT=w[:, kd, :], rhs=hT[:, kd, tok], start=(kd == 0), stop=(kd == KD - 1))
                    S.op("act", "activation", (PSB[pq], PRB), (QB[j],), out=qT[:, j, tok], in_=PS[pq][:], func=AF.Identity, bias=bqt[:, j, :], scale=0.125)
            smx = [al(f"smx{i}", [128, 256], F32) for i in range(2)]
            SMB = [Buf(), Buf()]
            ex = [al(f"ex{i}", [128, 256], BF16) for i in range(2)]
            EXB = [Buf(), Buf()]
            eT = [al(f"eT{i}", [128, 256], BF16) for i in range(2)]
            ETB = [Buf(), Buf()]
            stt = [al(f"stt{i}", [128, 8], F32) for i in range(4)]
            STTB = [Buf() for _ in range(4)]
            osw = [al(f"osw{i}", [128, 1024], F32) for i in range(2)]
            OSB = [Buf(), Buf()]
            ysw = [al(f"ysw{i}", [128, 1024], BF16) for i in range(2)]
            YSB = [Buf(), Buf()]
            junk2 = al("junk2", [128, 1024], F32)
            J2B = Buf()
            it = 0
            for n in range(NT):
                ob, obb = osw[n % 2], OSB[n % 2]
                for j in range(8):
                    for c in range(2):
                        head = j + 8 * c
                        kx, kxb = ((kTa, KAB), (kTb, KBB))[c]
                        if n == 0:
                            N_, keys, msk, vblk = 128, slice(0, 128), amask[:, 128:256], [0]
                        else:
                            N_, keys, msk, vblk = 256, slice((n - 1) * 128, (n + 1) * 128), amask[:, 0:256], [n - 1, n]
                        sm_, smb = smx[it % 2], SMB[it % 2]
                        e_, eb_ = ex[it % 2], EXB[it % 2]
                        t_, tb_ = eT[it % 2], ETB[it % 2]
                        s4, s4b = stt[it % 4], STTB[it % 4]
                        it += 1
                        (psc,) = ps_get()
                        S.op("pe", "matmul", (QB[j], kxb), (PSB[psc],), out=PS[psc][:, 0:N_], lhsT=qT[:, j, n * 128:(n + 1) * 128], rhs=kx[:, keys], start=True, stop=True)
                        S.op("dve", "tensor_tensor", (PSB[psc], CB), (smb,), out=sm_[:, 0:N_], in0=PS[psc][:, 0:N_], in1=msk, op=ALU.add)
                        S.op("dve", "reduce_max", (smb,), (s4b,), out=s4[:, 0:1], in_=sm_[:, 0:N_], axis=AX.X)
                        S.op("dve", "tensor_scalar", (s4b,), (s4b,), out=s4[:, 1:2], in0=s4[:, 0:1], scalar1=-1.0, scalar2=None, op0=ALU.mult)
                        S.op("act", "activation", (smb, s4b), (eb_, s4b), out=e_[:, 0:N_], in_=sm_[:, 0:N_], func=AF.Exp, bias=s4[:, 1:2], scale=1.0, accum_out=s4[:, 2:3])
                        S.op("act", "activation", (s4b, PRB), (s4b,), out=s4[:, 3:4], in_=s4[:, 0:1], func=AF.Exp, bias=sink_t[:, head:head + 1], scale=-1.0)
                        S.op("dve", "tensor_tensor", (s4b,), (s4b,), out=s4[:, 4:5], in0=s4[:, 2:3], in1=s4[:, 3:4], op=ALU.add)
                        S.op("dve", "reciprocal", (s4b,), (s4b,), out=s4[:, 5:6], in_=s4[:, 4:5])
                        (pt,) = ps_get()
                        ptv = PS[pt][:].bitcast(BF16)
                        for kb in range(N_ // 128):
                            S.op("pe", "transpose", (eb_,), (PSB[pt],), out=ptv[:, kb * 128:(kb + 1) * 128], in_=e_[:, kb * 128:(kb + 1) * 128], identity=ident_b[:])
                        if it % 2 == 0:
                            S.op("dve", "tensor_copy", (PSB[pt],), (tb_,), out=t_[:, 0:N_], in_=ptv[:, 0:N_])
                        else:
                            S.op("act", "copy", (PSB[pt],), (tb_,), out=t_[:, 0:N_], in_=ptv[:, 0:N_])
                        (po,) = ps_get()
                        for kb, vb in enumerate(vblk):
                            S.op("pe", "matmul", (tb_, VSB), (PSB[po],), out=PS[po][:, 0:64], lhsT=t_[:, kb * 128:(kb + 1) * 128], rhs=Vs[:, vb, c * 64:(c + 1) * 64], start=(kb == 0), stop=(kb == len(vblk) - 1))
                        S.op("act", "activation", (PSB[po], s4b), (obb,), out=ob[:, head * 64:(head + 1) * 64], in_=PS[po][:, 0:64], func=AF.Identity, scale=s4[:, 5:6])
                s4, s4b = stt[it % 4], STTB[it % 4]
                it += 1
                S.op("act", "activation", (obb,), (J2B, s4b), out=junk2[:], in_=ob[:], func=AF.Square, accum_out=s4[:, 0:1])
                S.op("dve", "tensor_scalar", (s4b,), (s4b,), out=s4[:, 1:2], in0=s4[:, 0:1], scalar1=1.0 / 1024, scalar2=EPS, op0=ALU.mult, op1=ALU.add)
                S.op("act", "activation", (s4b,), (s4b,), out=s4[:, 2:3], in_=s4[:, 1:2], func=AF.Sqrt)
                S.op("dve", "reciprocal", (s4b,), (s4b,), out=s4[:, 3:4], in_=s4[:, 2:3])
                yb_, ybb = ysw[n % 2], YSB[n % 2]
                S.op("dve", "scalar_tensor_tensor", (obb, s4b, PRB), (ybb,), out=yb_[:], in0=ob[:], scalar=s4[:, 3:4], in1=anw[:], op0=ALU.mult, op1=ALU.mult)
                S.dma("sp", (ybb,), (), out=mix_d[n * 128:(n + 1) * 128, 1024:2048], in_=yb_[:])

        if STOP not in ("ln", "hgrn"):
            release(0)
            if STOP == "swa":
                dcs = [arena[:, 0:1024].bitcast(BF16), arena[:, 1024:2048].bitcast(BF16)]
                dfs = [arena[:, 2048:4096], arena[:, 4096:6144]]
                dB = [Buf(), Buf(), Buf(), Buf()]
                for i in range(NT):
                    S.dma("sp", (), (dB[i % 2],), out=dcs[i % 2], in_=mix_d[i * 128:(i + 1) * 128, :])
                    S.op("act", "copy", (dB[i % 2],), (dB[2 + i % 2],), out=dfs[i % 2], in_=dcs[i % 2])
                    out_tk.append(S.dma("sp", (dB[2 + i % 2],), (), out=dbg[i * 128:(i + 1) * 128, :], in_=dfs[i % 2]))
        if STOP in ("full", "moe", "h1"):
            g4 = al("g4", [128, NT, 4], F32)
            rowk = sb("rowk", [128, NT * 4], I32)
            gdT = al("gdT", [128, NT, 128], F32)
            masks = al("masks", [128, NT, E], BF16)
            G4B, RKB, GDTB, MKB = Buf(), Buf(), Buf(), Buf()
            mark_p = atop[0]
            wo = al("wo", [128, KD, D], BF16)
            WOB = [Buf() for _ in range(4)]
            for g in range(4):
                S.dma("pool", (WOUTB,), (WOB[g],), out=wo[:, g * 4:(g + 1) * 4, :], in_=w_out[g * 512:(g + 1) * 512, :].rearrange("(k p) c -> p k c", p=128))
            bo = al("bo", [128, D], F32)
            BOB = Buf()
            load_bcast(bo, BOB, b_out[0:1, :], D)
            load_bcast(pw, PWB, ln1_w[0:1, :], D)
            load_bcast(pb, PBB, ln1_b[0:1, :], D)
            wr = al("wr", [128, KD, E], F32)
            brt = al("brt", [128, E], F32)
            WRB = Buf()
            S.dma("sp", (), (WRB,), out=wr[:, :, :], in_=w_router.rearrange("(k p) e -> p k e", p=128))
            S.dma("sp", (), (WRB,), out=brt[:, :], in_=b_router[0:1, :].partition_broadcast(128))
            padt = al("padt", [128, E * C // 128 * 16], I32)
            PDB = Buf()
            S.op("pool", "memset", (), (PDB,), ap=padt[:], constant=SEQ)
            SLOTB = Buf()
            S.dma("sp", (PDB,), (SLOTB,), out=slot_d[0:E * C, :].rearrange("(p n) o -> p (n o)", p=128), in_=padt[:])
            S.dma("sp", (CB,), (), out=h1b_d[SEQ:SEQ + 128, :], in_=zer_b[:])
            mt = [al(f"mt{i}", [128, D], BF16) for i in range(2)]
            mT = [al(f"mT{i}", [128, KD, 128], BF16) for i in range(2)]
            u_ = [al(f"u{i}", [128, D], F32) for i in range(2)]
            h0t = [al("h0t0", [128, D], F32)] * 2
            h1t = [al(f"h1t{i}", [128, D], F32) for i in range(2)]
            h1bt = [al(f"h1bt{i}", [128, D], BF16) for i in range(2)]
            h1T = al("h1T", [128, KD, 128], F32)
            MTB, MTTB, UB, H0TB, H1TB, H1BB = ([Buf(), Buf()] for _ in range(6))
            H0TB = [H0TB[0]] * 2
            H1TTB = Buf()
            lg = al("lg", [128, E], F32)
            v8 = al("v8", [128, 8], F32)
            i8 = al("i8", [128, 8], U32)
            i8f = al("i8f", [128, 8], F32)
            rs = al("rs", [128, 8], F32)
            e4 = al("e4", [128, 4], F32)
            rowf = al("rowf", [128, E], F32)
            ovf = al("ovf", [128, E], F32)
            oh = al("oh", [128, E], F32)
            jE = al("jE", [128, E], F32)
            rk = al("rk", [128, 4], F32)
            gd = al("gd", [128, 128], F32)
            RTB = Buf()
            for i in range(NT):
                p2 = i % 2
                rows = slice(i * 128, (i + 1) * 128)
                S.dma("sp", (), (MTB[p2],), out=mt[p2][:], in_=mix_d[rows, :])
                S.dma("sp", (), (H0TB[p2],), out=h0t[p2][:], in_=h0_d[rows, :])
                for g in range(4):
                    (pi,) = ps_get()
                    pv = PS[pi][:].bitcast(BF16)
                    for j in range(4):
                        kd = g * 4 + j
                        S.op("pe", "transpose", (MTB[p2],), (PSB[pi],), out=pv[:, j * 128:(j + 1) * 128], in_=mt[p2][:, kd * 128:(kd + 1) * 128], identity=ident_b[:])
                    S.op("dve" if g % 2 == 0 else "act", "tensor_copy" if g % 2 == 0 else "copy", (PSB[pi],), (MTTB[p2],),
                         out=mT[p2][:, g * 4:(g + 1) * 4, :], in_=pv[:, 0:512].rearrange("p (j t) -> p j t", j=4))
                banks = ps_get(4)
                for nb in range(4):
                    bk_ = banks[nb]
                    for kd in range(KD):
                        S.op("pe", "matmul", (MTTB[p2], WOB[kd // 4]), (PSB[bk_],), out=PS[bk_][:], lhsT=mT[p2][:, kd, :], rhs=wo[:, kd, nb * 512:(nb + 1) * 512], start=(kd == 0), stop=(kd == KD - 1))
                    cs = slice(nb * 512, (nb + 1) * 512)
                    S.op("dve", "tensor_tensor", (PSB[bk_], BOB), (UB[p2],), out=u_[p2][:, cs], in0=PS[bk_][:], in1=bo[:, cs], op=ALU.add)
                S.op("dve", "scalar_tensor_tensor", (H0TB[p2], UB[p2]), (UB[p2],), out=u_[p2][:], in0=h0t[p2][:], scalar=ALPHA, in1=u_[p2][:], op0=ALU.mult, op1=ALU.add)
                layer_norm(u_[p2], UB[p2], h1t[p2], H1TB[p2], pw, PWB, pb, PBB)
                S.dma("sp", (H1TB[p2],), (), out=h1_d[rows, :], in_=h1t[p2][:])
                S.op("act", "copy", (H1TB[p2],), (H1BB[p2],), out=h1bt[p2][:], in_=h1t[p2][:])
                S.dma("sp", (H1BB[p2],), (), out=h1b_d[rows, :], in_=h1bt[p2][:])
                for g in range(4):
                    (pi,) = ps_get()
                    for j in range(4):
                        kd = g * 4 + j
                        S.op("pe", "transpose", (H1TB[p2],), (PSB[pi],), out=PS[pi][:, j * 128:(j + 1) * 128], in_=h1t[p2][:, kd * 128:(kd + 1) * 128], identity=ident_f[:])
                    S.op("dve" if g % 2 == 0 else "act", "tensor_copy" if g % 2 == 0 else "copy", (PSB[pi],), (H1TTB,),
                         out=h1T[:, g * 4:(g + 1) * 4, :], in_=PS[pi][:, 0:512].rearrange("p (j t) -> p j t", j=4))
                (pl,) = ps_get()
                for kd in range(KD):
                    S.op("pe", "matmul", (H1TTB, WRB), (PSB[pl],), out=PS[pl][:, 0:E], lhsT=h1T[:, kd, :], rhs=wr[:, kd, :], start=(kd == 0), stop=(kd == KD - 1))
                R_ = (RTB,)
                S.op("dve", "tensor_tensor", (PSB[pl], WRB), R_, out=lg[:], in0=PS[pl][:, 0:E], in1=brt[:], op=ALU.add)
                S.op("dve", "max", R_, R_, out=v8[:], in_=lg[:])
                S.op("dve", "max_index", R_, R_, out=i8[:], in_max=v8[:], in_values=lg[:])
                S.op("dve", "tensor_scalar", R_, R_, out=rs[:, 0:1], in0=v8[:, 0:1], scalar1=-1.0, scalar2=None, op0=ALU.mult)
                S.op("act", "activation", R_, R_, out=e4[:], in_=v8[:, 0:4], func=AF.Exp, bias=rs[:, 0:1], scale=1.0, accum_out=rs[:, 1:2])
                S.op("dve", "reciprocal", R_, R_, out=rs[:, 2:3], in_=rs[:, 1:2])
                S.op("dve", "tensor_scalar", R_, R_ + (G4B,), out=g4[:, i, :], in0=e4[:], scalar1=rs[:, 2:3], scalar2=None, op0=ALU.mult)
                S.op("dve", "tensor_copy", R_, R_, out=i8f[:], in_=i8[:])
                S.op("dve", "tensor_scalar", R_, (MKB,), out=masks[:, i, :], in0=lg[:], scalar1=v8[:, 3:4], scalar2=None, op0=ALU.is_ge)
                (pp,) = ps_get()
                for j in range(i + 1):
                    S.op("pe", "matmul", (MKB, CB), (PSB[pp],), out=PS[pp][:, 0:E], lhsT=(lstrict if j == i else ones_b)[:], rhs=masks[:, j, :], start=(j == 0), stop=(j == i))
                S.op("dve", "tensor_tensor", (PSB[pp], CB), R_, out=rowf[:], in0=PS[pp][:, 0:E], in1=rowbase[:], op=ALU.add)
                S.op("dve", "tensor_scalar", (PSB[pp],), R_, out=ovf[:], in0=PS[pp][:, 0:E], scalar1=C - 0.5, scalar2=1.0, op0=ALU.is_ge, op1=ALU.mult)
                S.op("dve", "tensor_scalar", R_, R_, out=jE[:], in0=rowf[:], scalar1=-1.0, scalar2=float(E * C), op0=ALU.mult, op1=ALU.add)
                S.op("dve", "tensor_tensor", R_, R_, out=jE[:], in0=jE[:], in1=ovf[:], op=ALU.mult)
                S.op("dve", "tensor_tensor", R_, R_, out=rowf[:], in0=rowf[:], in1=jE[:], op=ALU.add)
                S.op("pool", "memset", (GDTB,), R_, ap=gd[:], constant=0.0)
                for k in range(4):
                    S.op("dve", "tensor_scalar", R_ + (CB,), R_, out=oh[:], in0=iota_e[:], scalar1=i8f[:, k:k + 1], scalar2=None, op0=ALU.is_equal)
                    S.op("dve", "tensor_tensor", R_, R_, out=jE[:], in0=oh[:], in1=rowf[:], op=ALU.mult)
                    S.op("dve", "reduce_sum", R_, R_, out=rk[:, k:k + 1], in_=jE[:], axis=AX.X)
                    S.op("dve", "scalar_tensor_tensor", R_ + (G4B,), R_, out=gd[:, 0:E], in0=oh[:], scalar=g4[:, i, k:k + 1], in1=gd[:, 0:E], op0=ALU.mult, op1=ALU.add)
                S.op("dve", "tensor_copy", R_, (RKB,), out=rowk[:, i * 4:(i + 1) * 4], in_=rk[:])
                (pg,) = ps_get()
                S.op("pe", "transpose", R_, (PSB[pg],), out=PS[pg][:, 0:128], in_=gd[:], identity=ident_f[:])
                S.op("act", "copy", (PSB[pg],), (GDTB,), out=gdT[:, i, :], in_=PS[pg][:, 0:128])
                for k in range(4):
                    S.dma("pool", (RKB, CB), (SLOTB,), meth="indirect_dma_start", out=slot_d[:, :], out_offset=IOA(ap=rowk[:, i * 4 + k:i * 4 + k + 1], axis=0),
                          in_=tokid[:, i * 16:(i + 1) * 16], in_offset=None)
            release(mark_p)
            if STOP == "h1":
                for i in range(NT):
                    out_tk.append(S.dma("sp", (), (), out=dbg[i * 128:(i + 1) * 128, :], in_=h1_d[i * 128:(i + 1) * 128, :]))

        if STOP in ("full", "moe"):
            FS = min(512, DFF)
            NS = DFF // FS
            FC = FS // 128
            NBLK = (E * KF + 127) // 128
            bgT = al("bgT", [128, NBLK * 128], F32)
            buT = al("buT", [128, NBLK * 128], F32)
            BGB = Buf()
            btmp = al("btmp", [128, 128], F32)
            BTB = Buf()
            for src, dst in ((b_gate, bgT), (b_up, buT)):
                for blk in range(NBLK):
                    nr = min(128, E * KF - blk * 128)
                    S.op("pool", "memset", (), (BTB,), ap=btmp[:], constant=0.0)
                    S.dma("sp", (), (BTB,), out=btmp[0:nr, :], in_=src[blk * 128: blk * 128 + nr, :])
                    (pi,) = ps_get()
                    S.op("pe", "transpose", (BTB,), (PSB[pi],), out=PS[pi][:, 0:128], in_=btmp[:], identity=ident_f[:])
                    S.op("act", "copy", (PSB[pi],), (BGB,), out=dst[:, blk * 128:(blk + 1) * 128], in_=PS[pi][:, 0:128])
            sidx = [sb(f"sidx{i}", [128, CT], I32) for i in range(2)]
            SIB = [Buf(), Buf()]
            xg = [al(f"xg{i}", [128, D], BF16) for i in range(2)]
            XGB = [Buf(), Buf()]
            xT = al("xT", [128, KD, C], BF16)
            XTB = Buf()
            actT = al("actT", [128, KF, C], BF16)
            ACB = Buf()
            NSL = 4
            slabs = [al(f"slab{i}", [128, KD * 512], BF16) for i in range(NSL)]
            SLB = [Buf() for _ in range(NSL)]
            sln = [0]
            ysb = [al(f"ysb{i}", [128, 512], F32) for i in range(3)]
            YB = [Buf() for _ in range(3)]
            yn = [0]
            gt_ = [al(f"gt{i}", [128, 512], F32) for i in range(2)]
            ut_ = [al(f"ut{i}", [128, 512], F32) for i in range(2)]
            st_ = [al(f"st{i}", [128, 512], F32) for i in range(2)]
            GTB, UTB, STB2 = [Buf(), Buf()], [Buf(), Buf()], [Buf(), Buf()]
            en = [0]
            ccs = [(c0, min(512, C - c0)) for c0 in range(0, C, 512)]
            for e in range(E):
                si, sib = sidx[e % 2], SIB[e % 2]
                S.dma("sp", (SLOTB,), (sib,), out=si[:, :], in_=slot_d[e * C:(e + 1) * C, 0:1].rearrange("(t p) o -> p (t o)", p=128), allow_slow_non_contiguous=True)
                for t in range(CT):
                    g_, gb_ = xg[t % 2], XGB[t % 2]
                    S.dma("pool", (sib,), (gb_,), meth="indirect_dma_start", out=g_[:], out_offset=None, in_=h1b_d[:, :], in_offset=IOA(ap=si[:, t:t + 1], axis=0))
                    for g in range(4):
                        (pi,) = ps_get()
                        pv = PS[pi][:].bitcast(BF16)
                        for j in range(4):
                            kd = g * 4 + j
                            S.op("pe", "transpose", (gb_,), (PSB[pi],), out=pv[:, j * 128:(j + 1) * 128], in_=g_[:, kd * 128:(kd + 1) * 128], identity=ident_b[:])
                        S.op("dve" if g % 2 == 0 else "act", "tensor_copy" if g % 2 == 0 else "copy", (PSB[pi],), (XTB,),
                             out=xT[:, g * 4:(g + 1) * 4, t * 128:(t + 1) * 128], in_=pv[:, 0:512].rearrange("p (j t) -> p j t", j=4))
                for s in range(NS):
                    sg, sgb = slabs[sln[0] % NSL], SLB[sln[0] % NSL]
                    su, sub = slabs[(sln[0] + 1) % NSL], SLB[(sln[0] + 1) % NSL]
                    sln[0] += 2
                    sgv = sg[:, 0:KD * FS].rearrange("p (k f) -> p k f", k=KD)
                    suv = su[:, 0:KD * FS].rearrange("p (k f) -> p k f", k=KD)
                    ep_, el_ = e // EP, e % EP
                    S.dma("pool", (), (sgb,), out=sgv, in_=w_gate[ep_][el_ * D:(el_ + 1) * D, s * FS:(s + 1) * FS].rearrange("(k p) f -> p k f", p=128))
                    S.dma("pool", (), (sub,), out=suv, in_=w_up[ep_][el_ * D:(el_ + 1) * D, s * FS:(s + 1) * FS].rearrange("(k p) f -> p k f", p=128))
                    for fc in range(FC):
                        kf = s * FC + fc
                        bcol = e * KF + kf
                        for c0, cw in ccs:
                            pg_, pu_ = ps_get(2)
                            for kd in range(KD):
                                S.op("pe", "matmul", (sgb, XTB), (PSB[pg_],), out=PS[pg_][:, 0:cw], lhsT=sgv[:, kd, fc * 128:(fc + 1) * 128], rhs=xT[:, kd, c0:c0 + cw], start=(kd == 0), stop=(kd == KD - 1))
                            for kd in range(KD):
                                S.op("pe", "matmul", (sub, XTB), (PSB[pu_],), out=PS[pu_][:, 0:cw], lhsT=suv[:, kd, fc * 128:(fc + 1) * 128], rhs=xT[:, kd, c0:c0 + cw], start=(kd == 0), stop=(kd == KD - 1))
                            q2 = en[0] % 2
                            en[0] += 1
                            S.op("dve", "tensor_scalar", (PSB[pg_], BGB), (GTB[q2],), out=gt_[q2][:, 0:cw], in0=PS[pg_][:, 0:cw], scalar1=bgT[:, bcol:bcol + 1], scalar2=7.0, op0=ALU.add, op1=ALU.min)
                            S.op("act", "activation", (GTB[q2],), (STB2[q2],), out=st_[q2][:, 0:cw], in_=gt_[q2][:, 0:cw], func=AF.Sigmoid, scale=1.702)
                            S.op("dve", "tensor_scalar", (PSB[pu_], BGB), (UTB[q2],), out=ut_[q2][:, 0:cw], in0=PS[pu_][:, 0:cw], scalar1=buT[:, bcol:bcol + 1], scalar2=7.0, op0=ALU.add, op1=ALU.min)
                            S.op("pool", "tensor_scalar", (UTB[q2],), (UTB[q2],), out=ut_[q2][:, 0:cw], in0=ut_[q2][:, 0:cw], scalar1=-7.0, scalar2=1.0, op0=ALU.max, op1=ALU.add)
                            S.op("pool", "tensor_tensor", (GTB[q2], STB2[q2]), (GTB[q2],), out=gt_[q2][:, 0:cw], in0=gt_[q2][:, 0:cw], in1=st_[q2][:, 0:cw], op=ALU.mult)
                            S.op("dve", "tensor_tensor", (GTB[q2], UTB[q2]), (ACB,), out=actT[:, kf, c0:c0 + cw], in0=gt_[q2][:, 0:cw], in1=ut_[q2][:, 0:cw], op=ALU.mult)
                for dn in range(4):
                    sd, sdb = slabs[sln[0] % NSL], SLB[sln[0] % NSL]
                    sln[0] += 1
                    sdv = sd[:, 0:KF * 512].rearrange("p (k m) -> p k m", k=KF)
                    ep_, el_ = e // EP, e % EP
                    S.dma("pool", (), (sdb,), out=sdv, in_=w_down[ep_][el_ * DFF:(el_ + 1) * DFF, dn * 512:(dn + 1) * 512].rearrange("(k p) m -> p k m", p=128))
                    for t in range(CT):
                        (py,) = ps_get()
                        for kf in range(KF):
                            S.op("pe", "matmul", (ACB, sdb), (PSB[py],), out=PS[py][:], lhsT=actT[:, kf, t * 128:(t + 1) * 128], rhs=sdv[:, kf, :], start=(kf == 0), stop=(kf == KF - 1))
                        y_, yb2 = ysb[yn[0] % 3], YB[yn[0] % 3]
                        yn[0] += 1
                        S.op("act" if yn[0] % 2 else "dve", "copy" if yn[0] % 2 else "tensor_copy", (PSB[py],), (yb2,), out=y_[:], in_=PS[py][:])
                        S.dma("sp", (yb2,), (), out=yall_d[e * C + t * 128: e * C + (t + 1) * 128, dn * 512:(dn + 1) * 512], in_=y_[:])
            release(mark_p)

            bd = al("bd", [128, D], F32)
            BDB = Buf()
            S.op("pool", "memset", (), (BDB,), ap=bd[:], constant=0.0)
            S.dma("sp", (), (BDB,), out=bd[0:E, :], in_=b_down[:, :])
            load_bcast(pw, PWB, ln2_w[0:1, :], D)
            load_bcast(pb, PBB, ln2_b[0:1, :], D)
            yk = [al(f"yk{i}", [128, D], F32) for i in range(4)]
            YKB = [Buf() for _ in range(4)]
            acc = [al(f"acc{i}", [128, D], F32) for i in range(2)]
            ACCB = [Buf(), Buf()]
            h1r = [al(f"h1r{i}", [128, D], F32) for i in range(2)]
            H1RB = [Buf(), Buf()]
            ot = [al(f"ot{i}", [128, D], F32) for i in range(2)]
            OTB = [Buf(), Buf()]
            for i in range(NT):
                p2 = i % 2
                rows = slice(i * 128, (i + 1) * 128)
                for k in range(4):
                    S.dma("pool", (RKB,), (YKB[k],), meth="indirect_dma_start", out=yk[k][:], out_offset=None, in_=yall_d[:, :],
                          in_offset=IOA(ap=rowk[:, i * 4 + k:i * 4 + k + 1], axis=0))
                S.dma("sp", (), (H1RB[p2],), out=h1r[p2][:], in_=h1_d[rows, :])
                banks = ps_get(4)
                for nb in range(4):
                    cs = slice(nb * 512, (nb + 1) * 512)
                    S.op("pe", "matmul", (GDTB, BDB), (PSB[banks[nb]],), out=PS[banks[nb]][:], lhsT=gdT[:, i, :], rhs=bd[:, cs], start=True, stop=True)
                    S.op("dve", "scalar_tensor_tensor", (YKB[0], G4B, PSB[banks[nb]]), (ACCB[p2],), out=acc[p2][:, cs], in0=yk[0][:, cs], scalar=g4[:, i, 0:1], in1=PS[banks[nb]][:], op0=ALU.mult, op1=ALU.add)
                for k in range(1, 4):
                    S.op("dve", "scalar_tensor_tensor", (YKB[k], G4B, ACCB[p2]), (ACCB[p2],), out=acc[p2][:], in0=yk[k][:], scalar=g4[:, i, k:k + 1], in1=acc[p2][:], op0=ALU.mult, op1=ALU.add)
                S.op("dve", "scalar_tensor_tensor", (H1RB[p2], ACCB[p2]), (ACCB[p2],), out=acc[p2][:], in0=h1r[p2][:], scalar=ALPHA, in1=acc[p2][:], op0=ALU.mult, op1=ALU.add)
                layer_norm(acc[p2], ACCB[p2], ot[p2], OTB[p2], pw, PWB, pb, PBB)
                out_tk.append(S.dma("sp", (OTB[p2],), (), out=(out if STOP == "full" else dbg)[rows, :], in_=ot[p2][:]))

        for q in ("sp",):
            S.wait_all(q, out_tk)
        block = st.enter_context(nc.Block())
        S.emit(block)
    return nc


FULL_CFG = dict(NCORES=8, E=32, ELOC=32, DFF=2048, C=384, SEQ=2048, NP=4)


def make_in_maps(cfg, inputs):
    NCORES, E, ELOC, DFF = cfg["NCORES"], cfg["E"], cfg["ELOC"], cfg["DFF"]
    f = lambda a: np.ascontiguousarray(np.asarray(a, dtype=np.float32))
    maps = []
    for c in range(NCORES):
        m = {
            "x": f(inputs["x"][c]),
            "ln_emb_w": f(inputs["ln_emb_w"]).reshape(1, D), "ln_emb_b": f(inputs["ln_emb_b"]).reshape(1, D),
            "w_in": f(inputs["w_in"][0]), "b_qkv": f(inputs["b_qkv"]).reshape(1, 1280),
            "lb_logits": f(inputs["lb_logits"]), "hg_norm_w": f(inputs["hg_norm_w"]).reshape(1, 128),
            "attn_sinks": f(inputs["attn_sinks"]).reshape(1, 16), "attn_norm_w": f(inputs["attn_norm_w"]).reshape(1, 1024),
            "w_out": f(inputs["w_out"][0]), "b_out": f(inputs["b_out"]).reshape(1, D),
            "ln1_w": f(inputs["ln1_w"]).reshape(1, D), "ln1_b": f(inputs["ln1_b"]).reshape(1, D),
            "w_router": f(inputs["w_router"][0]), "b_router": f(inputs["b_router"]).reshape(1, E),
            "b_gate": f(inputs["b_gate"][0]).reshape(E * DFF // 128, 128), "b_up": f(inputs["b_up"][0]).reshape(E * DFF // 128, 128),
            "b_down": f(inputs["b_down"][0]).reshape(E, D),
            "ln2_w": f(inputs["ln2_w"]).reshape(1, D), "ln2_b": f(inputs["ln2_b"]).reshape(1, D),
        }
        NP = cfg.get("NP", 1)
        EP = E // NP
        for p in range(NP):
            m[f"w_gate{p}"] = f(inputs["w_gate"][0][p * EP:(p + 1) * EP]).reshape(EP * D, DFF)
            m[f"w_up{p}"] = f(inputs["w_up"][0][p * EP:(p + 1) * EP]).reshape(EP * D, DFF)
            m[f"w_down{p}"] = f(inputs["w_down"][0][p * EP:(p + 1) * EP]).reshape(EP * DFF, D)
        maps.append(m)
    return maps


def kernel(**inputs):
    cfg = FULL_CFG
    nc = build(cfg)
    maps = make_in_maps(cfg, inputs)
    res = run_bass_kernel_spmd(nc, maps, core_ids=list(range(cfg["NCORES"])))
    return np.stack([np.asarray(r["out"], dtype=np.float32) for r in res.results], axis=0)
```

```python
import contextlib
import numpy as np
import concourse.bass as bass
import concourse.mybir as mybir
from concourse.bass_utils import run_bass_kernel_spmd

F32 = mybir.dt.float32
BF16 = mybir.dt.bfloat16
I32 = mybir.dt.int32
U32 = mybir.dt.uint32
AF = mybir.ActivationFunctionType
ALU = mybir.AluOpType
AX = mybir.AxisListType

D = 2048
KD = 16
ALPHA = 2.0 ** 0.25
EPS = 1e-5
NEG = -30000.0


class Buf:
    __slots__ = ("w", "r")

    def __init__(self):
        self.w = None
        self.r = {}


class Sched:
    ENGS = ("pe", "act", "dve", "pool", "sp")
    K = 8

    def __init__(self, nc, st, window=8):
        self.nc, self.st, self.window = nc, st, window
        self.prog = {e: [] for e in self.ENGS}
        self.sem, self.cnt, self.nsem = {}, {}, 0
        self.idx = {e: 0 for e in self.ENGS}
        self.seen = {e: {} for e in self.ENGS}
        self.dq = {}
        self.ndma = 0
        for e in ("pe", "act", "dve", "pool"):
            self._rot(e)

    def _newsem(self, nm):
        self.nsem += 1
        return self.st.enter_context(self.nc.semaphore(f"{nm}_{self.nsem}"))

    def _rot(self, e):
        self.sem[e] = self._newsem("s" + e)
        self.cnt[e] = 0

    def _deps(self, reads, writes):
        deps = []
        for b in reads:
            deps.append(b.w)
        for b in writes:
            deps.append(b.w)
            deps.extend(b.r.values())
        return deps

    def _waits(self, eng, deps):
        best = {}
        for t in deps:
            if t is None:
                continue
            sem, val, teng, tidx = t
            if teng == eng:
                if eng == "pe" or self.idx[eng] - tidx > self.window:
                    continue
            k = id(sem)
            if self.seen[eng].get(k, 0) >= val:
                continue
            if k not in best or best[k][1] < val:
                best[k] = (sem, val)
        out = []
        for k, (sem, val) in best.items():
            self.seen[eng][k] = val
            out.append((sem, val))
        return out

    def op(self, eng, meth, reads=(), writes=(), **kw):
        waits = self._waits(eng, self._deps(reads, writes))
        self.cnt[eng] += 1
        self.idx[eng] += 1
        tk = (self.sem[eng], self.cnt[eng], eng, self.idx[eng])
        self.prog[eng].append((waits, meth, kw, self.sem[eng], 1))
        if self.cnt[eng] >= 30000:
            self._rot(eng)
        for b in reads:
            b.r[eng] = tk
        for b in writes:
            b.w = tk
            b.r = {}
        return tk

    def dma(self, q, reads=(), writes=(), meth="dma_start", **kw):
        d = self.dq.get(q)
        if d is None:
            d = self.dq[q] = {"sems": [self._newsem("d" + q) for _ in range(self.K)], "cnt": [0] * self.K, "n": 0}
        i = d["n"] % self.K
        d["n"] += 1
        self.ndma += 1
        sem, prev = d["sems"][i], d["cnt"][i]
        deps = self._deps(reads, writes)
        if prev > 0:
            deps.append((sem, prev, None, 0))
        waits = self._waits(q, deps)
        d["cnt"][i] = prev + 16
        tk = (sem, prev + 16, None, 0)
        self.prog[q].append((waits, meth, kw, sem, 16))
        for b in reads:
            b.r[("dma", self.ndma)] = tk
        for b in writes:
            b.w = tk
            b.r = {}
        return tk

    def special(self, q, buf, meth, **kw):
        sem = self._newsem("x" + q)
        self.prog[q].append(([], meth, kw, sem, 16))
        buf.w = (sem, 16, None, 0)
        buf.r = {}

    def barrier(self):
        last = []
        for e in ("pe", "act", "dve", "pool"):
            if self.cnt[e] > 0:
                last.append((self.sem[e], self.cnt[e], e, self.idx[e]))
        for q, d in self.dq.items():
            for sem, c in zip(d["sems"], d["cnt"]):
                if c > 0:
                    last.append((sem, c, None, 0))
        for e in self.ENGS:
            deps = [t for t in last if t[2] != e]
            waits = self._waits(e, deps)
            if waits:
                self.prog[e].append((waits, None, None, None, 0))

    def wait_all(self, eng, tickets):
        waits = self._waits(eng, list(tickets))
        self.prog[eng].append((waits, None, None, None, 0))

    def emit(self, block):
        m = {"pe": block.tensor, "act": block.scalar, "dve": block.vector, "pool": block.gpsimd, "sp": block.sync}
        for e in self.ENGS:
            prog = self.prog[e]

            def body(eng, prog=prog):
                for waits, meth, kw, sem, inc in prog:
                    for s, v in waits:
                        eng.wait_ge(s, v)
                    if meth is not None:
                        try:
                            getattr(eng, meth)(**kw).then_inc(sem, inc)
                        except Exception:
                            print("FAILED OP", meth, {k: str(v)[:400] for k, v in kw.items()})
                            if meth == "indirect_dma_start":
                                print("IDX AP", kw["out_offset"].ap if kw.get("out_offset") is not None else None)
                                for drop in (("bounds_check", "oob_is_err"),):
                                    kw2 = {k: v for k, v in kw.items() if k not in drop}
                                    try:
                                        getattr(eng, meth)(**kw2)
                                        print("WORKS without", drop)
                                    except Exception as ex2:
                                        print("still fails without", drop, repr(ex2)[:200])
                            raise

            m[e](body)


def build(cfg):
    NCORES, E, ELOC, DFF, C, SEQ = cfg["NCORES"], cfg["E"], cfg["ELOC"], cfg["DFF"], cfg["C"], cfg["SEQ"]
    STOP = cfg.get("STOP", "full")
    NT = SEQ // 128
    KF = DFF // 128
    CT = C // 128
    TB = 512
    NTB = SEQ // TB
    nc = bass.Bass("TRN2", target_bir_lowering=False)

    def din(name, shape, dt=F32):
        return nc.dram_tensor(name, shape, dt, kind="ExternalInput").ap()

    def dscr(name, shape, dt):
        return nc.dram_tensor(name, shape, dt, kind="Internal").ap()

    x = din("x", [SEQ, D])
    ln_emb_w, ln_emb_b = din("ln_emb_w", [1, D]), din("ln_emb_b", [1, D])
    w_in = din("w_in", [D, 5376])
    b_qkv = din("b_qkv", [1, 1280])
    lb_logits = din("lb_logits", [2, 1024])
    hg_norm_w = din("hg_norm_w", [1, 128])
    attn_sinks = din("attn_sinks", [1, 16])
    attn_norm_w = din("attn_norm_w", [1, 1024])
    w_out = din("w_out", [D, D])
    b_out = din("b_out", [1, D])
    ln1_w, ln1_b = din("ln1_w", [1, D]), din("ln1_b", [1, D])
    w_router = din("w_router", [D, E])
    b_router = din("b_router", [1, E])
    NP = cfg.get("NP", 1)
    EP = E // NP
    w_gate = [din(f"w_gate{p}", [EP * D, DFF]) for p in range(NP)]
    w_up = [din(f"w_up{p}", [EP * D, DFF]) for p in range(NP)]
    w_down = [din(f"w_down{p}", [EP * DFF, D]) for p in range(NP)]
    b_gate = din("b_gate", [E * KF, 128])
    b_up = din("b_up", [E * KF, 128])
    b_down = din("b_down", [E, D])
    ln2_w, ln2_b = din("ln2_w", [1, D]), din("ln2_b", [1, D])
    out = nc.dram_tensor("out", [SEQ, D], F32, kind="ExternalOutput").ap()
    dbg = None
    if STOP != "full":
        dbg = nc.dram_tensor("dbg", [SEQ, D], F32, kind="ExternalOutput").ap()

    h0_d = dscr("h0_d", [SEQ, D], F32)
    mix_d = dscr("mix_d", [SEQ, D], BF16)
    h1_d = dscr("h1_d", [SEQ, D], F32)
    h1b_d = dscr("h1b_d", [SEQ + 128, D], BF16)
    slot_d = dscr("slot_d", [E * C + 128, 16], I32)
    yall_d = dscr("yall_d", [E * C + 128, D], F32)
    with contextlib.ExitStack() as st:
        S = Sched(nc, st)

        def sb(name, shape, dt):
            return st.enter_context(nc.sbuf_tensor(name, shape, dt))

        ARENA_W = 43008
        arena = sb("arena", [128, ARENA_W], F32)
        atop = [0]

        def al(name, shape, dt):
            esz = 4 if dt in (F32, I32, U32) else 2
            n = 1
            for d_ in shape[1:]:
                n *= d_
            words = (n * esz + 3) // 4
            words = (words + 7) // 8 * 8
            a0 = atop[0]
            atop[0] += words
            assert atop[0] <= ARENA_W, (name, atop[0])
            v = arena[:, a0:a0 + words]
            if dt != F32:
                v = v.bitcast(dt)
            v = v[:, 0:n]
            if len(shape) == 3:
                v = v.rearrange("p (a b) -> p a b", a=shape[1])
            return v

        def release(mark):
            S.barrier()
            atop[0] = mark

        PS = [st.enter_context(nc.psum_tensor(f"ps{i}", [128, 512], F32)) for i in range(8)]
        PSB = [Buf() for _ in range(8)]
        psn = [0]

        def ps_get(n=1):
            if (psn[0] % 8) + n > 8:
                psn[0] += 8 - (psn[0] % 8)
            i = psn[0] % 8
            psn[0] += n
            return list(range(i, i + n))

        ident_f = sb("ident_f", [128, 128], F32)
        ident_b = sb("ident_b", [128, 128], BF16)
        ones_b = sb("ones_b", [128, 128], BF16)
        lstrict = sb("lstrict", [128, 128], BF16)
        hmask = sb("hmask", [128, 128], F32)
        amask = sb("amask", [128, 256], F32)
        m01 = sb("m01", [128, TB], F32)
        iota_e = sb("iota_e", [128, E], F32)
        rowbase = sb("rowbase", [128, E], F32)
        tokid = sb("tokid", [128, NT * 16], I32)
        padidx = sb("padidx", [128, 1], I32)
        zer_b = sb("zer_b", [128, D], BF16)
        CB = Buf()
        P = lambda meth, **kw: S.op("pool", meth, (), (CB,), **kw)
        P("memset", ap=ident_f[:], constant=1.0)
        P("affine_select", out=ident_f[:], in_=ident_f[:], pattern=[[-1, 128]], compare_op=ALU.is_equal, fill=0.0, base=0, channel_multiplier=1)
        P("tensor_copy", out=ident_b[:], in_=ident_f[:])
        P("memset", ap=ones_b[:], constant=1.0)
        P("memset", ap=hmask[:], constant=1.0)
        P("affine_select", out=hmask[:], in_=hmask[:], pattern=[[1, 128]], compare_op=ALU.is_ge, fill=0.0, base=-1, channel_multiplier=-1)
        P("tensor_copy", out=lstrict[:], in_=hmask[:])
        P("memset", ap=hmask[:], constant=1.0)
        P("affine_select", out=hmask[:], in_=hmask[:], pattern=[[1, 128]], compare_op=ALU.is_ge, fill=0.0, base=0, channel_multiplier=-1)
        P("memset", ap=hmask[0:64, 64:128], constant=0.0)
        P("memset", ap=amask[:], constant=0.0)
        P("affine_select", out=amask[:], in_=amask[:], pattern=[[1, 256]], compare_op=ALU.is_ge, fill=NEG, base=-1, channel_multiplier=-1)
        P("affine_select", out=amask[:], in_=amask[:], pattern=[[-1, 256]], compare_op=ALU.is_ge, fill=NEG, base=128, channel_multiplier=1)
        P("memset", ap=m01[:], constant=1.0)
        P("memset", ap=m01[:].rearrange("p (c t) -> p c t", t=64)[:, :, 0:1], constant=0.0)
        P("iota", out=iota_e[:], pattern=[[1, E]], base=0, channel_multiplier=0, allow_small_or_imprecise_dtypes=True)
        P("iota", out=rowbase[:], pattern=[[C, E]], base=0, channel_multiplier=0, allow_small_or_imprecise_dtypes=True)
        P("iota", out=tokid[:], pattern=[[128, NT], [0, 16]], base=0, channel_multiplier=1)
        P("memset", ap=padidx[:], constant=SEQ)
        P("memset", ap=zer_b[:], constant=0.0)

        pw = sb("pw", [128, D], F32)
        pb = sb("pb", [128, D], F32)
        PWB, PBB = Buf(), Buf()

        def load_bcast(dst, dbuf, src_row, n):
            S.dma("sp", (), (dbuf,), out=dst[:, 0:n], in_=src_row.partition_broadcast(128))

        stats = sb("stats", [128, 4, 6], F32)
        mv = sb("mv", [128, 2], F32)
        sm1 = sb("sm1", [128, 8], F32)
        STB = Buf()

        def layer_norm(src, sbuf, dst, dbuf, w_t, w_b, b_t, b_b):
            for c in range(4):
                S.op("dve", "bn_stats", (sbuf,), (STB,), out=stats[:, c, :], in_=src[:, c * 512:(c + 1) * 512])
            S.op("dve", "bn_aggr", (STB,), (STB,), out=mv[:], in_=stats[:].rearrange("p a b -> p (a b)"))
            S.op("dve", "tensor_scalar", (STB,), (STB,), out=sm1[:, 0:1], in0=mv[:, 1:2], scalar1=EPS, scalar2=None, op0=ALU.add)
            S.op("act", "activation", (STB,), (STB,), out=sm1[:, 1:2], in_=sm1[:, 0:1], func=AF.Sqrt)
            S.op("dve", "reciprocal", (STB,), (STB,), out=sm1[:, 2:3], in_=sm1[:, 1:2])
            S.op("dve", "scalar_tensor_tensor", (STB,), (STB,), out=sm1[:, 3:4], in0=mv[:, 0:1], scalar=-1.0, in1=sm1[:, 2:3], op0=ALU.mult, op1=ALU.mult)
            S.op("act", "activation", (sbuf, STB), (dbuf,), out=dst[:], in_=src[:], func=AF.Identity, bias=sm1[:, 3:4], scale=sm1[:, 2:3])
            S.op("dve", "tensor_tensor", (dbuf, w_b), (dbuf,), out=dst[:], in0=dst[:], in1=w_t[:], op=ALU.mult)
            S.op("pool", "tensor_tensor", (dbuf, b_b), (dbuf,), out=dst[:], in0=dst[:], in1=b_t[:], op=ALU.add)

        hT = al("hT", [128, KD, SEQ], BF16)
        mark_hT = atop[0]
        HTB = [Buf() for _ in range(NT)]
        tA = [al(f"tA{i}", [128, D], F32) for i in range(3)]
        tAB = [Buf(), Buf(), Buf()]
        tB = [al(f"tB{i}", [128, D], F32) for i in range(3)]
        tBB = [Buf(), Buf(), Buf()]
        tC = [al(f"tC{i}", [128, D], BF16) for i in range(3)]
        tCB = [Buf(), Buf(), Buf()]

        out_tk = []

        WINB, WOUTB = Buf(), Buf()

        load_bcast(pw, PWB, ln_emb_w[0:1, :], D)
        load_bcast(pb, PBB, ln_emb_b[0:1, :], D)
        H0B = [Buf() for _ in range(NT)]
        for i in range(NT):
            a, ab = tA[i % 3], tAB[i % 3]
            b, bb = tB[i % 3], tBB[i % 3]
            c, cb = tC[i % 3], tCB[i % 3]
            S.dma("sp", (), (ab,), out=a[:], in_=x[i * 128:(i + 1) * 128, :])
            layer_norm(a, ab, b, bb, pw, PWB, pb, PBB)
            S.dma("sp", (bb,), (H0B[i],), out=h0_d[i * 128:(i + 1) * 128, :], in_=b[:])
            S.op("act", "activation", (bb,), (cb,), out=c[:], in_=b[:], func=AF.Copy)
            for g in range(4):
                (pi,) = ps_get()
                pv = PS[pi][:].bitcast(BF16)
                for j in range(4):
                    kd = g * 4 + j
                    S.op("pe", "transpose", (cb,), (PSB[pi],), out=pv[:, j * 128:(j + 1) * 128], in_=c[:, kd * 128:(kd + 1) * 128], identity=ident_b[:])
                S.op("dve" if g % 2 == 0 else "act", "tensor_copy" if g % 2 == 0 else "copy", (PSB[pi],), (HTB[i],),
                     out=hT[:, g * 4:(g + 1) * 4, i * 128:(i + 1) * 128],
                     in_=pv[:, 0:512].rearrange("p (j t) -> p j t", j=4))
        if STOP == "ln":
            for i in range(NT):
                out_tk.append(S.dma("sp", (H0B[i],), (), out=dbg[i * 128:(i + 1) * 128, :], in_=h0_d[i * 128:(i + 1) * 128, :]))

        release(mark_hT)
        lbt = sb("lbt", [128, 8, 4], F32)
        LBB = Buf()
        if STOP not in ("ln",):
            S.dma("sp", (), (LBB,), out=lbt[:, :, 0:1], in_=lb_logits[0:1, :].rearrange("o (h p) -> p h o", p=128), allow_slow_non_contiguous=True)
            S.dma("sp", (), (LBB,), out=lbt[:, :, 1:2], in_=lb_logits[1:2, :].rearrange("o (h p) -> p h o", p=128), allow_slow_non_contiguous=True)
            S.op("dve", "tensor_tensor", (LBB,), (LBB,), out=lbt[:, :, 2:3], in0=lbt[:, :, 0:1], in1=lbt[:, :, 1:2], op=ALU.subtract)
            S.op("act", "activation", (LBB,), (LBB,), out=lbt[:, :, 2:3], in_=lbt[:, :, 2:3], func=AF.Sigmoid)
            S.op("dve", "tensor_scalar", (LBB,), (LBB,), out=lbt[:, :, 3:4], in0=lbt[:, :, 2:3], scalar1=-1.0, scalar2=1.0, op0=ALU.mult, op1=ALU.add)
            hgw = sb("hgw", [128, 128], F32)
            HGWB = Buf()
            S.dma("sp", (), (HGWB,), out=hgw[:], in_=hg_norm_w[0:1, :].partition_broadcast(128))

            wh = [al(f"wh{i}", [128, KD, 512], BF16) for i in range(2)]
            WHB = [Buf(), Buf()]
            QT = al("QT", [128, SEQ], BF16)
            KT = al("KT", [128, SEQ], BF16)
            QTB, KTB = Buf(), Buf()
            QTa = al("QTa", [128, SEQ], BF16)
            QTb = al("QTb", [128, SEQ], BF16)
            QTaB, QTbB = Buf(), Buf()
            Vt0 = al("Vt0", [128, NT, 128], BF16)
            Vt1 = al("Vt1", [128, NT, 128], BF16)
            Vt0B, Vt1B = Buf(), Buf()
            S.op("pool", "memset", (), (QTaB,), ap=QTa[:], constant=0.0)
            S.op("pool", "memset", (), (QTbB,), ap=QTb[:], constant=0.0)
            S.op("pool", "memset", (), (Vt0B,), ap=Vt0[:], constant=0.0)
            S.op("pool", "memset", (), (Vt1B,), ap=Vt1[:], constant=0.0)
            EBL = al("EBL", [128, SEQ // 64], F32)
            EBLB = Buf()
            Ktm = al("Ktm", [128, NT, 128], BF16)
            Vtm = al("Vtm", [128, NT, 128], BF16)
            GW = al("GW", [128, NT, 128], F32)
            Yh = al("Yh", [128, NT, 128], BF16)
            KtmB, VtmB, GWB, YhB = Buf(), Buf(), Buf(), Buf()
            ft = [al(f"ft{i}", [128, TB], F32) for i in range(6)]
            FTB = [Buf() for _ in range(6)]
            Sf = [al(f"Sf{i}", [128, 128], F32) for i in range(2)]
            Sb_ = [al(f"Sb{i}", [128, 128], BF16) for i in range(2)]
            SfB = [Buf(), Buf()]
            SbB = [Buf(), Buf()]
            sT = [al(f"sT{i}", [128, 128], BF16) for i in range(2)]
            sTB = [Buf(), Buf()]
            tmpS = al("tmpS", [128, 128], F32)
            tmpSB = Buf()
            hsm = sb("hsm", [128, 8], F32)
            hsm2 = sb("hsm2", [128, 8], F32)
            HSMB2 = Buf()
            HSMB = Buf()
            junk = al("junk", [128, 128], F32)
            JB = Buf()

            NH = cfg.get("NH", 8)
            for hh in range(NH):
                w, wb = wh[hh % 2], WHB[hh % 2]
                for gi, base in enumerate((0, 1024, 2048, 3072)):
                    S.dma("pool", (WINB,), (wb,), out=w[:, :, gi * 128:(gi + 1) * 128],
                          in_=w_in[:, base + hh * 128: base + (hh + 1) * 128].rearrange("(k p) c -> p k c", p=128))
                for tb in range(NTB):
                    tok = slice(tb * TB, (tb + 1) * TB)
                    pq, pf = ps_get(2)
                    for kd in range(KD):
                        S.op("pe", "matmul", (wb,) + tuple(HTB[tb * 4:(tb + 1) * 4]), (PSB[pq],), out=PS[pq][:], lhsT=w[:, kd, 0:128], rhs=hT[:, kd, tok], start=(kd == 0), stop=(kd == KD - 1))
                    for kd in range(KD):
                        S.op("pe", "matmul", (wb,) + tuple(HTB[tb * 4:(tb + 1) * 4]), (PSB[pf],), out=PS[pf][:], lhsT=w[:, kd, 128:256], rhs=hT[:, kd, tok], start=(kd == 0), stop=(kd == KD - 1))
                    sig, f, lf, bb_, eb, enb = ft
                    S.op("act", "activation", (PSB[pf],), (FTB[0],), out=sig[:], in_=PS[pf][:], func=AF.Sigmoid)
                    S.op("dve", "tensor_scalar", (FTB[0], LBB), (FTB[1],), out=f[:], in0=sig[:], scalar1=lbt[:, hh, 3:4], scalar2=lbt[:, hh, 2:3], op0=ALU.mult, op1=ALU.add)
                    S.op("act", "activation", (FTB[1],), (FTB[2],), out=lf[:], in_=f[:], func=AF.Ln)
                    S.op("dve", "tensor_tensor_scan", (FTB[2], CB), (FTB[3],), out=bb_[:], data0=m01[:], data1=lf[:], initial=0.0, op0=ALU.mult, op1=ALU.add)
                    S.op("act", "activation", (FTB[3],), (FTB[4],), out=eb[:], in_=bb_[:], func=AF.Exp)
                    S.op("act", "activation", (FTB[3],), (FTB[5],), out=enb[:], in_=bb_[:], func=AF.Exp, scale=-1.0)
                    S.op("pool", "tensor_copy", (FTB[4],), (EBLB,), out=EBL[:, tb * 8:(tb + 1) * 8], in_=eb[:].rearrange("p (c t) -> p c t", t=64)[:, :, 63])
                    S.op("act", "activation", (PSB[pq],), (FTB[0],), out=sig[:], in_=PS[pq][:], func=AF.Sigmoid)
                    S.op("dve", "tensor_tensor", (FTB[0], PSB[pq]), (FTB[0],), out=sig[:], in0=sig[:], in1=PS[pq][:], op=ALU.mult)
                    S.op("dve", "tensor_tensor", (FTB[0], FTB[4]), (QTB,), out=QT[:, tok], in0=sig[:], in1=eb[:], op=ALU.mult)
                    qv = lambda t_: t_[:, tok].rearrange("p (c two t) -> p c two t", two=2, t=64)
                    S.op("pool", "tensor_copy", (QTB,), (QTaB,), out=qv(QTa)[:, :, 0, :], in_=qv(QT)[:, :, 0, :])
                    S.op("pool", "tensor_copy", (QTB,), (QTbB,), out=qv(QTb)[:, :, 1, :], in_=qv(QT)[:, :, 1, :])
                    S.op("pool", "tensor_scalar", (FTB[1],), (FTB[2],), out=lf[:], in0=f[:], scalar1=-1.0, scalar2=1.0, op0=ALU.mult, op1=ALU.add)
                    S.op("pool", "tensor_tensor", (FTB[2], FTB[5]), (KTB,), out=KT[:, tok], in0=lf[:], in1=enb[:], op=ALU.mult)
                HS = cfg.get("HSTOP", 3)
                for i in range(NT if HS >= 2 else 0):
                    (pv_,) = ps_get()
                    for kd in range(KD):
                        S.op("pe", "matmul", (wb, HTB[i]), (PSB[pv_],), out=PS[pv_][:, 0:256], lhsT=hT[:, kd, i * 128:(i + 1) * 128], rhs=w[:, kd, 256:512], start=(kd == 0), stop=(kd == KD - 1))
                    S.op("act", "copy", (PSB[pv_],), (VtmB,), out=Vtm[:, i, :], in_=PS[pv_][:, 0:128])
                    S.op("act", "copy", (PSB[pv_],), (Vt0B,), out=Vt0[0:64, i, :], in_=PS[pv_][0:64, 0:128])
                    S.op("act", "copy", (PSB[pv_],), (Vt1B,), out=Vt1[64:128, i, :], in_=PS[pv_][64:128, 0:128])
                    S.op("act", "activation", (PSB[pv_],), (JB,), out=junk[:], in_=PS[pv_][:, 128:256], func=AF.Sigmoid)
                    S.op("dve", "tensor_tensor", (JB, PSB[pv_]), (JB,), out=junk[:], in0=junk[:], in1=PS[pv_][:, 128:256], op=ALU.mult)
                    S.op("pool", "tensor_tensor", (JB, HGWB), (GWB,), out=GW[:, i, :], in0=junk[:], in1=hgw[:], op=ALU.mult)
                    (pt,) = ps_get()
                    ptv = PS[pt][:].bitcast(BF16)
                    S.op("pe", "transpose", (KTB,), (PSB[pt],), out=ptv[:, 0:128], in_=KT[:, i * 128:(i + 1) * 128], identity=ident_b[:])
                    S.op("dve", "tensor_copy", (PSB[pt],), (KtmB,), out=Ktm[:, i, :], in_=ptv[:, 0:128])
                S.op("pool", "memset", (), (SfB[0],), ap=Sf[0][:], constant=0.0)
                S.op("pool", "memset", (), (SbB[0],), ap=Sb_[0][:], constant=0.0)
                hsms = [hsm, hsm2]
                hsmbs = [HSMB, HSMB2]
                stA = {}

                def stage_a(i):
                    tk_ = slice(i * 128, (i + 1) * 128)
                    (psc,) = ps_get()
                    S.op("pe", "matmul", (KTB, QTB), (PSB[psc],), out=PS[psc][:, 0:128], lhsT=KT[:, tk_], rhs=QT[:, tk_], start=True, stop=True)
                    s_, s_b = sT[i % 2], sTB[i % 2]
                    S.op("dve", "tensor_tensor", (PSB[psc], CB), (s_b,), out=s_[:], in0=PS[psc][:, 0:128], in1=hmask[:], op=ALU.mult)
                    pps = []
                    for c in range(2):
                        (pp,) = ps_get()
                        S.op("pe", "matmul", (KtmB, Vt0B, Vt1B), (PSB[pp],), out=PS[pp][:, 0:128], lhsT=Ktm[:, i, :], rhs=(Vt0, Vt1)[c][:, i, :], start=True, stop=True)
                        pps.append(pp)
                    stA[i] = pps

                def stage_b(i, cur):
                    tk_ = slice(i * 128, (i + 1) * 128)
                    s_, s_b = sT[i % 2], sTB[i % 2]
                    pps = stA.pop(i)
                    (po,) = ps_get()
                    S.op("pe", "matmul", (s_b, VtmB), (PSB[po],), out=PS[po][:, 0:128], lhsT=s_[:], rhs=Vtm[:, i, :], start=True, stop=False)
                    for c in range(2):
                        S.op("pe", "matmul", (QTaB, QTbB, SbB[cur]), (PSB[po],), out=PS[po][:, 0:128], lhsT=(QTa, QTb)[c][:, tk_], rhs=Sb_[cur][:], start=False, stop=(c == 1))
                        pp = pps[c]
                        nxt = 1 - cur
                        S.op("dve", "tensor_tensor", (PSB[pp], SfB[cur]), (tmpSB,), out=tmpS[:], in0=PS[pp][:, 0:128], in1=Sf[cur][:], op=ALU.add)
                        S.op("dve", "tensor_scalar", (tmpSB, EBLB), (SfB[nxt],), out=Sf[nxt][:], in0=tmpS[:], scalar1=EBL[:, 2 * i + c: 2 * i + c + 1], scalar2=None, op0=ALU.mult)
                        S.op("act", "copy", (SfB[nxt],), (SbB[nxt],), out=Sb_[nxt][:], in_=Sf[nxt][:])
                        cur = nxt
                    hs_, hb_ = hsms[i % 2], hsmbs[i % 2]
                    S.op("act", "activation", (PSB[po],), (JB, hb_), out=junk[:], in_=PS[po][:, 0:128], func=AF.Square, accum_out=hs_[:, 0:1])
                    S.op("dve", "tensor_scalar", (hb_,), (hb_,), out=hs_[:, 1:2], in0=hs_[:, 0:1], scalar1=1.0 / 128, scalar2=EPS, op0=ALU.mult, op1=ALU.add)
                    S.op("act", "activation", (hb_,), (hb_,), out=hs_[:, 2:3], in_=hs_[:, 1:2], func=AF.Sqrt)
                    S.op("dve", "reciprocal", (hb_,), (hb_,), out=hs_[:, 3:4], in_=hs_[:, 2:3])
                    S.op("dve", "scalar_tensor_tensor", (PSB[po], hb_, GWB), (YhB,), out=Yh[:, i, :], in0=PS[po][:, 0:128], scalar=hs_[:, 3:4], in1=GW[:, i, :], op0=ALU.mult, op1=ALU.mult)
                    return cur

                cur = 0
                if HS >= 3:
                    stage_a(0)
                    for i in range(NT):
                        if i + 1 < NT:
                            stage_a(i + 1)
                        cur = stage_b(i, cur)
                S.dma("sp", (YhB,), (), out=mix_d[:, hh * 128:(hh + 1) * 128].rearrange("(i p) v -> p i v", p=128), in_=Yh[:])
            if STOP == "hgrn":
                S.op("pool", "tensor_copy", (YhB,), (tAB[0],), out=tA[0][:, 0:NT * 128].rearrange("p (i v) -> p i v", v=128), in_=Yh[:])
                out_tk.append(S.dma("sp", (tAB[0],), (), out=dbg[0:128, 0:NT * 128], in_=tA[0][:, 0:NT * 128]))

        IOA = bass.IndirectOffsetOnAxis
        if STOP not in ("ln", "hgrn"):
            release(mark_hT)
            wq = [al(f"wq{i}", [128, KD, 128], BF16) for i in range(2)]
            WQB = [Buf(), Buf()]
            wkv = al("wkv", [128, KD, 256], BF16)
            WKVB = Buf()
            qT = al("qT", [128, 8, SEQ], BF16)
            QB = [Buf() for _ in range(8)]
            kTa = al("kTa", [128, SEQ], BF16)
            kTb = al("kTb", [128, SEQ], BF16)
            KAB, KBB = Buf(), Buf()
            Vs = al("Vs", [128, NT, 128], BF16)
            VSB = Buf()
            bqt = al("bqt", [128, 8, 1], F32)
            bkt = al("bkt", [128, 1, 1], F32)
            bvb = al("bvb", [128, 128], F32)
            sink_t = al("sink_t", [128, 16], F32)
            anw = al("anw", [128, 1024], F32)
            PRB = Buf()
            S.dma("sp", (), (PRB,), out=bqt[0:64, :, :], in_=b_qkv[0:1, 0:512].rearrange("o (j p) -> p j o", p=64), allow_slow_non_contiguous=True)
            S.dma("sp", (), (PRB,), out=bqt[64:128, :, :], in_=b_qkv[0:1, 512:1024].rearrange("o (j p) -> p j o", p=64), allow_slow_non_contiguous=True)
            S.dma("sp", (), (PRB,), out=bkt[:, :, :], in_=b_qkv[0:1, 1024:1152].rearrange("o (j p) -> p j o", p=128), allow_slow_non_contiguous=True)
            S.dma("sp", (), (PRB,), out=bvb[:, :], in_=b_qkv[0:1, 1152:1280].partition_broadcast(128))
            S.dma("sp", (), (PRB,), out=sink_t[:, :], in_=attn_sinks[0:1, :].partition_broadcast(128))
            S.dma("sp", (), (PRB,), out=anw[:, :], in_=attn_norm_w[0:1, :].partition_broadcast(128))
            S.op("dve", "tensor_scalar", (PRB,), (PRB,), out=bqt[:, :, 0], in0=bqt[:, :, 0], scalar1=0.125, scalar2=None, op0=ALU.mult)
            S.op("pool", "memset", (), (KAB,), ap=kTa[:], constant=0.0)
            S.op("pool", "memset", (), (KBB,), ap=kTb[:], constant=0.0)
            for half, c0 in ((0, 5120), (1, 5248)):
                S.dma("pool", (WINB,), (WKVB,), out=wkv[:, :, half * 128:(half + 1) * 128], in_=w_in[:, c0:c0 + 128].rearrange("(k p) c -> p k c", p=128))
            allh = tuple(HTB)
            for tb in range(NTB):
                tok = slice(tb * TB, (tb + 1) * TB)
                (pk,) = ps_get()
                for kd in range(KD):
                    S.op("pe", "matmul", (WKVB,) + allh, (PSB[pk],), out=PS[pk][:], lhsT=wkv[:, kd, 0:128], rhs=hT[:, kd, tok], start=(kd == 0), stop=(kd == KD - 1))
                S.op("act", "activation", (PSB[pk], PRB), (KAB,), out=kTa[0:64, tok], in_=PS[pk][0:64, :], func=AF.Identity, bias=bkt[0:64, 0, :], scale=1.0)
                S.op("act", "activation", (PSB[pk], PRB), (KBB,), out=kTb[64:128, tok], in_=PS[pk][64:128, :], func=AF.Identity, bias=bkt[64:128, 0, :], scale=1.0)
            for i in range(NT):
                (pv_,) = ps_get()
                for kd in range(KD):
                    S.op("pe", "matmul", (WKVB,) + allh, (PSB[pv_],), out=PS[pv_][:, 0:128], lhsT=hT[:, kd, i * 128:(i + 1) * 128], rhs=wkv[:, kd, 128:256], start=(kd == 0), stop=(kd == KD - 1))
                S.op("dve", "tensor_tensor", (PSB[pv_], PRB), (VSB,), out=Vs[:, i, :], in0=PS[pv_][:, 0:128], in1=bvb[:], op=ALU.add)
            for j in range(8):
                w, wb = wq[j % 2], WQB[j % 2]
                for half, hd in ((0, j), (1, 8 + j)):
                    S.dma("pool", (WINB,), (wb,), out=w[:, :, half * 64:(half + 1) * 64], in_=w_in[:, 4096 + hd * 64: 4096 + (hd + 1) * 64].rearrange("(k p) c -> p k c", p=128))
                for tb in range(NTB):
                    tok = slice(tb * TB, (tb + 1) * TB)
                    (pq,) = ps_get()
                    for kd in range(KD):
                        S.op("pe", "matmul", (wb,) + allh, (PSB[pq],), out=PS[pq][:], lhsT=w[:, kd, :], rhs=hT[:, kd, tok], start=(kd == 0), stop=(kd == KD - 1))
                    S.op("act", "activation", (PSB[pq], PRB), (QB[j],), out=qT[:, j, tok], in_=PS[pq][:], func=AF.Identity, bias=bqt[:, j, :], scale=0.125)
            smx = [al(f"smx{i}", [128, 256], F32) for i in range(2)]
            SMB = [Buf(), Buf()]
            ex = [al(f"ex{i}", [128, 256], BF16) for i in range(2)]
            EXB = [Buf(), Buf()]
            eT = [al(f"eT{i}", [128, 256], BF16) for i in range(2)]
            ETB = [Buf(), Buf()]
            stt = [al(f"stt{i}", [128, 8], F32) for i in range(4)]
            STTB = [Buf() for _ in range(4)]
            osw = [al(f"osw{i}", [128, 1024], F32) for i in range(2)]
            OSB = [Buf(), Buf()]
            ysw = [al(f"ysw{i}", [128, 1024], BF16) for i in range(2)]
            YSB = [Buf(), Buf()]
            junk2 = al("junk2", [128, 1024], F32)
            J2B = Buf()
            stt2 = al("stt2", [128, 8], F32)
            STT2B = Buf()
            def swa_iter(n, j, c, it):
                ob, obb = osw[n % 2], OSB[n % 2]
                head = j + 8 * c
                kx, kxb = ((kTa, KAB), (kTb, KBB))[c]
                if n == 0:
                    N_, keys, msk, vblk = 128, slice(0, 128), amask[:, 128:256], [0]
                else:
                    N_, keys, msk, vblk = 256, slice((n - 1) * 128, (n + 1) * 128), amask[:, 0:256], [n - 1, n]
                sm_, smb = smx[it % 2], SMB[it % 2]
                e_, eb_ = ex[it % 2], EXB[it % 2]
                t_, tb_ = eT[it % 2], ETB[it % 2]
                s4, s4b = stt[it % 4], STTB[it % 4]
                loc = {}

                def s0():
                    (psc,) = ps_get()
                    loc["psc"] = psc
                    S.op("pe", "matmul", (QB[j], kxb), (PSB[psc],), out=PS[psc][:, 0:N_], lhsT=qT[:, j, n * 128:(n + 1) * 128], rhs=kx[:, keys], start=True, stop=True)

                def s1():
                    psc = loc["psc"]
                    S.op("dve", "tensor_tensor", (PSB[psc], CB), (smb,), out=sm_[:, 0:N_], in0=PS[psc][:, 0:N_], in1=msk, op=ALU.add)
                    S.op("dve", "reduce_max", (smb,), (s4b,), out=s4[:, 0:1], in_=sm_[:, 0:N_], axis=AX.X)
                    S.op("dve", "tensor_scalar", (s4b,), (s4b,), out=s4[:, 1:2], in0=s4[:, 0:1], scalar1=-1.0, scalar2=None, op0=ALU.mult)

                def s2():
                    S.op("act", "activation", (smb, s4b), (eb_, s4b), out=e_[:, 0:N_], in_=sm_[:, 0:N_], func=AF.Exp, bias=s4[:, 1:2], scale=1.0, accum_out=s4[:, 2:3])
                    S.op("act", "activation", (s4b, PRB), (s4b,), out=s4[:, 3:4], in_=s4[:, 0:1], func=AF.Exp, bias=sink_t[:, head:head + 1], scale=-1.0)

                def s3():
                    (pt,) = ps_get()
                    loc["pt"] = pt
                    ptv = PS[pt][:].bitcast(BF16)
                    for kb in range(N_ // 128):
                        S.op("pe", "transpose", (eb_,), (PSB[pt],), out=ptv[:, kb * 128:(kb + 1) * 128], in_=e_[:, kb * 128:(kb + 1) * 128], identity=ident_b[:])
                    S.op("dve", "tensor_tensor", (s4b,), (s4b,), out=s4[:, 4:5], in0=s4[:, 2:3], in1=s4[:, 3:4], op=ALU.add)
                    S.op("dve", "reciprocal", (s4b,), (s4b,), out=s4[:, 5:6], in_=s4[:, 4:5])

                def s4_():
                    pt = loc["pt"]
                    ptv = PS[pt][:].bitcast(BF16)
                    if it % 2 == 0:
                        S.op("dve", "tensor_copy", (PSB[pt],), (tb_,), out=t_[:, 0:N_], in_=ptv[:, 0:N_])
                    else:
                        S.op("act", "copy", (PSB[pt],), (tb_,), out=t_[:, 0:N_], in_=ptv[:, 0:N_])

                def s5():
                    (po,) = ps_get()
                    for kb, vb in enumerate(vblk):
                        S.op("pe", "matmul", (tb_, VSB), (PSB[po],), out=PS[po][:, 0:64], lhsT=t_[:, kb * 128:(kb + 1) * 128], rhs=Vs[:, vb, c * 64:(c + 1) * 64], start=(kb == 0), stop=(kb == len(vblk) - 1))
                    S.op("act", "activation", (PSB[po], s4b), (obb,), out=ob[:, head * 64:(head + 1) * 64], in_=PS[po][:, 0:64], func=AF.Identity, scale=s4[:, 5:6])

                return [s0, s1, s2, s3, s4_, s5]

            it = 0
            for n in range(NT):
                ob, obb = osw[n % 2], OSB[n % 2]
                for j in range(8):
                    sa = swa_iter(n, j, 0, it)
                    sb_ = swa_iter(n, j, 1, it + 1)
                    it += 2
                    for fa, fb in zip(sa, sb_):
                        fa()
                        fb()
                s4, s4b = stt2, STT2B
                S.op("act", "activation", (obb,), (J2B, s4b), out=junk2[:], in_=ob[:], func=AF.Square, accum_out=s4[:, 0:1])
                S.op("dve", "tensor_scalar", (s4b,), (s4b,), out=s4[:, 1:2], in0=s4[:, 0:1], scalar1=1.0 / 1024, scalar2=EPS, op0=ALU.mult, op1=ALU.add)
                S.op("act", "activation", (s4b,), (s4b,), out=s4[:, 2:3], in_=s4[:, 1:2], func=AF.Sqrt)
                S.op("dve", "reciprocal", (s4b,), (s4b,), out=s4[:, 3:4], in_=s4[:, 2:3])
                yb_, ybb = ysw[n % 2], YSB[n % 2]
                S.op("dve", "scalar_tensor_tensor", (obb, s4b, PRB), (ybb,), out=yb_[:], in0=ob[:], scalar=s4[:, 3:4], in1=anw[:], op0=ALU.mult, op1=ALU.mult)
                S.dma("sp", (ybb,), (), out=mix_d[n * 128:(n + 1) * 128, 1024:2048], in_=yb_[:])

        if STOP not in ("ln", "hgrn"):
            release(0)
            if STOP == "swa":
                dcs = [arena[:, 0:1024].bitcast(BF16), arena[:, 1024:2048].bitcast(BF16)]
                dfs = [arena[:, 2048:4096], arena[:, 4096:6144]]
                dB = [Buf(), Buf(), Buf(), Buf()]
                for i in range(NT):
                    S.dma("sp", (), (dB[i % 2],), out=dcs[i % 2], in_=mix_d[i * 128:(i + 1) * 128, :])
                    S.op("act", "copy", (dB[i % 2],), (dB[2 + i % 2],), out=dfs[i % 2], in_=dcs[i % 2])
                    out_tk.append(S.dma("sp", (dB[2 + i % 2],), (), out=dbg[i * 128:(i + 1) * 128, :], in_=dfs[i % 2]))
        if STOP in ("full", "moe", "h1"):
            g4 = al("g4", [128, NT, 4], F32)
            rowk = sb("rowk", [128, NT * 4], I32)
            gdT = al("gdT", [128, NT, 128], F32)
            masks = al("masks", [128, NT, E], BF16)
            G4B, RKB, GDTB, MKB = Buf(), Buf(), Buf(), Buf()
            mark_p = atop[0]
            wo = al("wo", [128, KD, D], BF16)
            WOB = [Buf() for _ in range(4)]
            for g in range(4):
                S.dma("pool", (WOUTB,), (WOB[g],), out=wo[:, g * 4:(g + 1) * 4, :], in_=w_out[g * 512:(g + 1) * 512, :].rearrange("(k p) c -> p k c", p=128))
            bo = al("bo", [128, D], F32)
            BOB = Buf()
            load_bcast(bo, BOB, b_out[0:1, :], D)
            load_bcast(pw, PWB, ln1_w[0:1, :], D)
            load_bcast(pb, PBB, ln1_b[0:1, :], D)
            wr = al("wr", [128, KD, E], F32)
            brt = al("brt", [128, E], F32)
            WRB = Buf()
            S.dma("sp", (), (WRB,), out=wr[:, :, :], in_=w_router.rearrange("(k p) e -> p k e", p=128))
            S.dma("sp", (), (WRB,), out=brt[:, :], in_=b_router[0:1, :].partition_broadcast(128))
            padt = al("padt", [128, E * C // 128 * 16], I32)
            PDB = Buf()
            S.op("pool", "memset", (), (PDB,), ap=padt[:], constant=SEQ)
            SLOTB = Buf()
            S.dma("sp", (PDB,), (SLOTB,), out=slot_d[0:E * C, :].rearrange("(p n) o -> p (n o)", p=128), in_=padt[:])
            S.dma("sp", (CB,), (), out=h1b_d[SEQ:SEQ + 128, :], in_=zer_b[:])
            mt = [al(f"mt{i}", [128, D], BF16) for i in range(2)]
            mT = [al(f"mT{i}", [128, KD, 128], BF16) for i in range(2)]
            u_ = [al(f"u{i}", [128, D], F32) for i in range(2)]
            h0t = [al("h0t0", [128, D], F32)] * 2
            h1t = [al(f"h1t{i}", [128, D], F32) for i in range(2)]
            h1bt = [al(f"h1bt{i}", [128, D], BF16) for i in range(2)]
            h1T = al("h1T", [128, KD, 128], F32)
            MTB, MTTB, UB, H0TB, H1TB, H1BB = ([Buf(), Buf()] for _ in range(6))
            H0TB = [H0TB[0]] * 2
            H1TTB = Buf()
            lg = al("lg", [128, E], F32)
            v8 = al("v8", [128, 8], F32)
            i8 = al("i8", [128, 8], U32)
            i8f = al("i8f", [128, 8], F32)
            rs = al("rs", [128, 8], F32)
            e4 = al("e4", [128, 4], F32)
            rowf = al("rowf", [128, E], F32)
            ovf = al("ovf", [128, E], F32)
            oh = al("oh", [128, E], F32)
            jE = al("jE", [128, E], F32)
            rk = al("rk", [128, 4], F32)
            gd = al("gd", [128, 128], F32)
            RTB = Buf()
            def front(i):
                p2 = i % 2
                rows = slice(i * 128, (i + 1) * 128)
                S.dma("sp", (), (MTB[p2],), out=mt[p2][:], in_=mix_d[rows, :])
                S.dma("sp", (), (H0TB[p2],), out=h0t[p2][:], in_=h0_d[rows, :])
                for g in range(4):
                    (pi,) = ps_get()
                    pv = PS[pi][:].bitcast(BF16)
                    for j in range(4):
                        kd = g * 4 + j
                        S.op("pe", "transpose", (MTB[p2],), (PSB[pi],), out=pv[:, j * 128:(j + 1) * 128], in_=mt[p2][:, kd * 128:(kd + 1) * 128], identity=ident_b[:])
                    S.op("dve" if g % 2 == 0 else "act", "tensor_copy" if g % 2 == 0 else "copy", (PSB[pi],), (MTTB[p2],),
                         out=mT[p2][:, g * 4:(g + 1) * 4, :], in_=pv[:, 0:512].rearrange("p (j t) -> p j t", j=4))
                banks = ps_get(4)
                for nb in range(4):
                    bk_ = banks[nb]
                    for kd in range(KD):
                        S.op("pe", "matmul", (MTTB[p2], WOB[kd // 4]), (PSB[bk_],), out=PS[bk_][:], lhsT=mT[p2][:, kd, :], rhs=wo[:, kd, nb * 512:(nb + 1) * 512], start=(kd == 0), stop=(kd == KD - 1))
                    cs = slice(nb * 512, (nb + 1) * 512)
                    S.op("dve", "tensor_tensor", (PSB[bk_], BOB), (UB[p2],), out=u_[p2][:, cs], in0=PS[bk_][:], in1=bo[:, cs], op=ALU.add)
                S.op("dve", "scalar_tensor_tensor", (H0TB[p2], UB[p2]), (UB[p2],), out=u_[p2][:], in0=h0t[p2][:], scalar=ALPHA, in1=u_[p2][:], op0=ALU.mult, op1=ALU.add)

            def back(i):
                p2 = i % 2
                rows = slice(i * 128, (i + 1) * 128)
                layer_norm(u_[p2], UB[p2], h1t[p2], H1TB[p2], pw, PWB, pb, PBB)
                S.dma("sp", (H1TB[p2],), (), out=h1_d[rows, :], in_=h1t[p2][:])
                S.op("act", "copy", (H1TB[p2],), (H1BB[p2],), out=h1bt[p2][:], in_=h1t[p2][:])
                S.dma("sp", (H1BB[p2],), (), out=h1b_d[rows, :], in_=h1bt[p2][:])
                for g in range(4):
                    (pi,) = ps_get()
                    for j in range(4):
                        kd = g * 4 + j
                        S.op("pe", "transpose", (H1TB[p2],), (PSB[pi],), out=PS[pi][:, j * 128:(j + 1) * 128], in_=h1t[p2][:, kd * 128:(kd + 1) * 128], identity=ident_f[:])
                    S.op("dve" if g % 2 == 0 else "act", "tensor_copy" if g % 2 == 0 else "copy", (PSB[pi],), (H1TTB,),
                         out=h1T[:, g * 4:(g + 1) * 4, :], in_=PS[pi][:, 0:512].rearrange("p (j t) -> p j t", j=4))
                (pl,) = ps_get()
                for kd in range(KD):
                    S.op("pe", "matmul", (H1TTB, WRB), (PSB[pl],), out=PS[pl][:, 0:E], lhsT=h1T[:, kd, :], rhs=wr[:, kd, :], start=(kd == 0), stop=(kd == KD - 1))
                R_ = (RTB,)
                S.op("dve", "tensor_tensor", (PSB[pl], WRB), R_, out=lg[:], in0=PS[pl][:, 0:E], in1=brt[:], op=ALU.add)
                S.op("dve", "max", R_, R_, out=v8[:], in_=lg[:])
                S.op("dve", "max_index", R_, R_, out=i8[:], in_max=v8[:], in_values=lg[:])
                S.op("dve", "tensor_scalar", R_, R_, out=rs[:, 0:1], in0=v8[:, 0:1], scalar1=-1.0, scalar2=None, op0=ALU.mult)
                S.op("act", "activation", R_, R_, out=e4[:], in_=v8[:, 0:4], func=AF.Exp, bias=rs[:, 0:1], scale=1.0, accum_out=rs[:, 1:2])
                S.op("dve", "reciprocal", R_, R_, out=rs[:, 2:3], in_=rs[:, 1:2])
                S.op("dve", "tensor_scalar", R_, R_ + (G4B,), out=g4[:, i, :], in0=e4[:], scalar1=rs[:, 2:3], scalar2=None, op0=ALU.mult)
                S.op("dve", "tensor_copy", R_, R_, out=i8f[:], in_=i8[:])
                S.op("dve", "tensor_scalar", R_, (MKB,), out=masks[:, i, :], in0=lg[:], scalar1=v8[:, 3:4], scalar2=None, op0=ALU.is_ge)
                (pp,) = ps_get()
                for j in range(i + 1):
                    S.op("pe", "matmul", (MKB, CB), (PSB[pp],), out=PS[pp][:, 0:E], lhsT=(lstrict if j == i else ones_b)[:], rhs=masks[:, j, :], start=(j == 0), stop=(j == i))
                S.op("dve", "tensor_tensor", (PSB[pp], CB), R_, out=rowf[:], in0=PS[pp][:, 0:E], in1=rowbase[:], op=ALU.add)
                S.op("dve", "tensor_scalar", (PSB[pp],), R_, out=ovf[:], in0=PS[pp][:, 0:E], scalar1=C - 0.5, scalar2=1.0, op0=ALU.is_ge, op1=ALU.mult)
                S.op("dve", "tensor_scalar", R_, R_, out=jE[:], in0=rowf[:], scalar1=-1.0, scalar2=float(E * C), op0=ALU.mult, op1=ALU.add)
                S.op("dve", "tensor_tensor", R_, R_, out=jE[:], in0=jE[:], in1=ovf[:], op=ALU.mult)
                S.op("dve", "tensor_tensor", R_, R_, out=rowf[:], in0=rowf[:], in1=jE[:], op=ALU.add)
                S.op("pool", "memset", (GDTB,), R_, ap=gd[:], constant=0.0)
                for k in range(4):
                    S.op("dve", "tensor_scalar", R_ + (CB,), R_, out=oh[:], in0=iota_e[:], scalar1=i8f[:, k:k + 1], scalar2=None, op0=ALU.is_equal)
                    S.op("dve", "tensor_tensor", R_, R_, out=jE[:], in0=oh[:], in1=rowf[:], op=ALU.mult)
                    S.op("dve", "reduce_sum", R_, R_, out=rk[:, k:k + 1], in_=jE[:], axis=AX.X)
                    S.op("dve", "scalar_tensor_tensor", R_ + (G4B,), R_, out=gd[:, 0:E], in0=oh[:], scalar=g4[:, i, k:k + 1], in1=gd[:, 0:E], op0=ALU.mult, op1=ALU.add)
                S.op("dve", "tensor_copy", R_, (RKB,), out=rowk[:, i * 4:(i + 1) * 4], in_=rk[:])
                (pg,) = ps_get()
                S.op("pe", "transpose", R_, (PSB[pg],), out=PS[pg][:, 0:128], in_=gd[:], identity=ident_f[:])
                S.op("act", "copy", (PSB[pg],), (GDTB,), out=gdT[:, i, :], in_=PS[pg][:, 0:128])
                for k in range(4):
                    S.dma("pool", (RKB, CB), (SLOTB,), meth="indirect_dma_start", out=slot_d[:, :], out_offset=IOA(ap=rowk[:, i * 4 + k:i * 4 + k + 1], axis=0),
                          in_=tokid[:, i * 16:(i + 1) * 16], in_offset=None)
            front(0)
            for i in range(NT):
                if i + 1 < NT:
                    front(i + 1)
                back(i)
            release(mark_p)
            if STOP == "h1":
                for i in range(NT):
                    out_tk.append(S.dma("sp", (), (), out=dbg[i * 128:(i + 1) * 128, :], in_=h1_d[i * 128:(i + 1) * 128, :]))

        if STOP in ("full", "moe"):
            FS = min(512, DFF)
            NS = DFF // FS
            FC = FS // 128
            NBLK = (E * KF + 127) // 128
            bgT = al("bgT", [128, NBLK * 128], F32)
            buT = al("buT", [128, NBLK * 128], F32)
            BGB = Buf()
            btmp = al("btmp", [128, 128], F32)
            BTB = Buf()
            for src, dst in ((b_gate, bgT), (b_up, buT)):
                for blk in range(NBLK):
                    nr = min(128, E * KF - blk * 128)
                    S.op("pool", "memset", (), (BTB,), ap=btmp[:], constant=0.0)
                    S.dma("sp", (), (BTB,), out=btmp[0:nr, :], in_=src[blk * 128: blk * 128 + nr, :])
                    (pi,) = ps_get()
                    S.op("pe", "transpose", (BTB,), (PSB[pi],), out=PS[pi][:, 0:128], in_=btmp[:], identity=ident_f[:])
                    S.op("act", "copy", (PSB[pi],), (BGB,), out=dst[:, blk * 128:(blk + 1) * 128], in_=PS[pi][:, 0:128])
            sidx = [sb(f"sidx{i}", [128, CT], I32) for i in range(2)]
            SIB = [Buf(), Buf()]
            xg = [al(f"xg{i}", [128, D], BF16) for i in range(2)]
            XGB = [Buf(), Buf()]
            xT = al("xT", [128, KD, C], BF16)
            XTB = Buf()
            actT = al("actT", [128, KF, C], BF16)
            ACB = Buf()
            NSL = cfg.get("NSL", 5)
            slabs = [al(f"slab{i}", [128, KD * 512], BF16) for i in range(NSL)]
            SLB = [Buf() for _ in range(NSL)]
            sln = [0]
            ysb = [al(f"ysb{i}", [128, 512], F32) for i in range(3)]
            YB = [Buf() for _ in range(3)]
            yn = [0]
            gt_ = [al(f"gt{i}", [128, 512], F32) for i in range(2)]
            ut_ = [al(f"ut{i}", [128, 512], F32) for i in range(2)]
            st_ = [al(f"st{i}", [128, 512], F32) for i in range(2)]
            GTB, UTB, STB2 = [Buf(), Buf()], [Buf(), Buf()], [Buf(), Buf()]
            en = [0]
            ccs = [(c0, min(512, C - c0)) for c0 in range(0, C, 512)]
            xT2 = al("xT2", [128, KD, C], BF16)
            XTB2 = Buf()
            xTs, XTBs = [xT, xT2], [XTB, XTB2]

            def prep(e):
                xT, XTB = xTs[e % 2], XTBs[e % 2]
                si, sib = sidx[e % 2], SIB[e % 2]
                S.dma("sp", (SLOTB,), (sib,), out=si[:, :], in_=slot_d[e * C:(e + 1) * C, 0:1].rearrange("(t p) o -> p (t o)", p=128), allow_slow_non_contiguous=True)
                for t in range(CT):
                    g_, gb_ = xg[t % 2], XGB[t % 2]
                    S.dma("pool", (sib,), (gb_,), meth="indirect_dma_start", out=g_[:], out_offset=None, in_=h1b_d[:, :], in_offset=IOA(ap=si[:, t:t + 1], axis=0))
                    for g in range(4):
                        (pi,) = ps_get()
                        pv = PS[pi][:].bitcast(BF16)
                        for j in range(4):
                            kd = g * 4 + j
                            S.op("pe", "transpose", (gb_,), (PSB[pi],), out=pv[:, j * 128:(j + 1) * 128], in_=g_[:, kd * 128:(kd + 1) * 128], identity=ident_b[:])
                        S.op("dve" if g % 2 == 0 else "act", "tensor_copy" if g % 2 == 0 else "copy", (PSB[pi],), (XTB,),
                             out=xT[:, g * 4:(g + 1) * 4, t * 128:(t + 1) * 128], in_=pv[:, 0:512].rearrange("p (j t) -> p j t", j=4))
            def gateup(e):
                xT, XTB = xTs[e % 2], XTBs[e % 2]
                for s in range(NS):
                    sg, sgb = slabs[sln[0] % NSL], SLB[sln[0] % NSL]
                    su, sub = slabs[(sln[0] + 1) % NSL], SLB[(sln[0] + 1) % NSL]
                    sln[0] += 2
                    sgv = sg[:, 0:KD * FS].rearrange("p (k f) -> p k f", k=KD)
                    suv = su[:, 0:KD * FS].rearrange("p (k f) -> p k f", k=KD)
                    ep_, el_ = e // EP, e % EP
                    S.dma("pool", (), (sgb,), out=sgv, in_=w_gate[ep_][el_ * D:(el_ + 1) * D, s * FS:(s + 1) * FS].rearrange("(k p) f -> p k f", p=128))
                    S.dma("pool", (), (sub,), out=suv, in_=w_up[ep_][el_ * D:(el_ + 1) * D, s * FS:(s + 1) * FS].rearrange("(k p) f -> p k f", p=128))
                    for fc in range(FC):
                        kf = s * FC + fc
                        bcol = e * KF + kf
                        for c0, cw in ccs:
                            pg_, pu_ = ps_get(2)
                            for kd in range(KD):
                                S.op("pe", "matmul", (sgb, XTB), (PSB[pg_],), out=PS[pg_][:, 0:cw], lhsT=sgv[:, kd, fc * 128:(fc + 1) * 128], rhs=xT[:, kd, c0:c0 + cw], start=(kd == 0), stop=(kd == KD - 1))
                            for kd in range(KD):
                                S.op("pe", "matmul", (sub, XTB), (PSB[pu_],), out=PS[pu_][:, 0:cw], lhsT=suv[:, kd, fc * 128:(fc + 1) * 128], rhs=xT[:, kd, c0:c0 + cw], start=(kd == 0), stop=(kd == KD - 1))
                            q2 = en[0] % 2
                            en[0] += 1
                            S.op("dve", "tensor_scalar", (PSB[pg_], BGB), (GTB[q2],), out=gt_[q2][:, 0:cw], in0=PS[pg_][:, 0:cw], scalar1=bgT[:, bcol:bcol + 1], scalar2=7.0, op0=ALU.add, op1=ALU.min)
                            S.op("act", "activation", (GTB[q2],), (STB2[q2],), out=st_[q2][:, 0:cw], in_=gt_[q2][:, 0:cw], func=AF.Sigmoid, scale=1.702)
                            S.op("dve", "tensor_scalar", (PSB[pu_], BGB), (UTB[q2],), out=ut_[q2][:, 0:cw], in0=PS[pu_][:, 0:cw], scalar1=buT[:, bcol:bcol + 1], scalar2=7.0, op0=ALU.add, op1=ALU.min)
                            S.op("dve", "tensor_scalar", (UTB[q2],), (UTB[q2],), out=ut_[q2][:, 0:cw], in0=ut_[q2][:, 0:cw], scalar1=-7.0, scalar2=1.0, op0=ALU.max, op1=ALU.add)
                            S.op("dve", "tensor_tensor", (GTB[q2], STB2[q2]), (GTB[q2],), out=gt_[q2][:, 0:cw], in0=gt_[q2][:, 0:cw], in1=st_[q2][:, 0:cw], op=ALU.mult)
                            S.op("dve", "tensor_tensor", (GTB[q2], UTB[q2]), (ACB,), out=actT[:, kf, c0:c0 + cw], in0=gt_[q2][:, 0:cw], in1=ut_[q2][:, 0:cw], op=ALU.mult)
            def down(e):
                for dn in range(4):
                    sd, sdb = slabs[sln[0] % NSL], SLB[sln[0] % NSL]
                    sln[0] += 1
                    sdv = sd[:, 0:KF * 512].rearrange("p (k m) -> p k m", k=KF)
                    ep_, el_ = e // EP, e % EP
                    S.dma("pool", (), (sdb,), out=sdv, in_=w_down[ep_][el_ * DFF:(el_ + 1) * DFF, dn * 512:(dn + 1) * 512].rearrange("(k p) m -> p k m", p=128))
                    for t in range(CT):
                        (py,) = ps_get()
                        for kf in range(KF):
                            S.op("pe", "matmul", (ACB, sdb), (PSB[py],), out=PS[py][:], lhsT=actT[:, kf, t * 128:(t + 1) * 128], rhs=sdv[:, kf, :], start=(kf == 0), stop=(kf == KF - 1))
                        y_, yb2 = ysb[yn[0] % 3], YB[yn[0] % 3]
                        yn[0] += 1
                        S.op("act" if yn[0] % 2 else "dve", "copy" if yn[0] % 2 else "tensor_copy", (PSB[py],), (yb2,), out=y_[:], in_=PS[py][:])
                        S.dma("sp", (yb2,), (), out=yall_d[e * C + t * 128: e * C + (t + 1) * 128, dn * 512:(dn + 1) * 512], in_=y_[:])
            prep(0)
            for e in range(E):
                gateup(e)
                if e + 1 < E:
                    prep(e + 1)
                down(e)
            release(mark_p)

            bd = al("bd", [128, D], F32)
            BDB = Buf()
            S.op("pool", "memset", (), (BDB,), ap=bd[:], constant=0.0)
            S.dma("sp", (), (BDB,), out=bd[0:E, :], in_=b_down[:, :])
            load_bcast(pw, PWB, ln2_w[0:1, :], D)
            load_bcast(pb, PBB, ln2_b[0:1, :], D)
            yk8 = [al(f"yk{i}", [128, D], F32) for i in range(8)]
            YKB8 = [Buf() for _ in range(8)]
            acc = [al(f"acc{i}", [128, D], F32) for i in range(2)]
            ACCB = [Buf(), Buf()]
            h1r = [al(f"h1r{i}", [128, D], F32) for i in range(2)]
            H1RB = [Buf(), Buf()]
            ot = [al(f"ot{i}", [128, D], F32) for i in range(2)]
            OTB = [Buf(), Buf()]
            for i in range(NT):
                p2 = i % 2
                rows = slice(i * 128, (i + 1) * 128)
                yk = yk8[p2 * 4:(p2 + 1) * 4]
                YKB = YKB8[p2 * 4:(p2 + 1) * 4]
                for k in range(4):
                    S.dma("pool", (RKB,), (YKB[k],), meth="indirect_dma_start", out=yk[k][:], out_offset=None, in_=yall_d[:, :],
                          in_offset=IOA(ap=rowk[:, i * 4 + k:i * 4 + k + 1], axis=0))
                S.dma("sp", (), (H1RB[p2],), out=h1r[p2][:], in_=h1_d[rows, :])
                banks = ps_get(4)
                for nb in range(4):
                    cs = slice(nb * 512, (nb + 1) * 512)
                    S.op("pe", "matmul", (GDTB, BDB), (PSB[banks[nb]],), out=PS[banks[nb]][:], lhsT=gdT[:, i, :], rhs=bd[:, cs], start=True, stop=True)
                    S.op("dve", "scalar_tensor_tensor", (YKB[0], G4B, PSB[banks[nb]]), (ACCB[p2],), out=acc[p2][:, cs], in0=yk[0][:, cs], scalar=g4[:, i, 0:1], in1=PS[banks[nb]][:], op0=ALU.mult, op1=ALU.add)
                for k in range(1, 4):
                    S.op("dve", "scalar_tensor_tensor", (YKB[k], G4B, ACCB[p2]), (ACCB[p2],), out=acc[p2][:], in0=yk[k][:], scalar=g4[:, i, k:k + 1], in1=acc[p2][:], op0=ALU.mult, op1=ALU.add)
                S.op("dve", "scalar_tensor_tensor", (H1RB[p2], ACCB[p2]), (ACCB[p2],), out=acc[p2][:], in0=h1r[p2][:], scalar=ALPHA, in1=acc[p2][:], op0=ALU.mult, op1=ALU.add)
                layer_norm(acc[p2], ACCB[p2], ot[p2], OTB[p2], pw, PWB, pb, PBB)
                out_tk.append(S.dma("sp", (OTB[p2],), (), out=(out if STOP == "full" else dbg)[rows, :], in_=ot[p2][:]))

        for q in ("sp",):
            S.wait_all(q, out_tk)
        block = st.enter_context(nc.Block())
        S.emit(block)
    return nc


FULL_CFG = dict(NCORES=8, E=32, ELOC=32, DFF=2048, C=384, SEQ=2048, NP=4)


def make_in_maps(cfg, inputs):
    NCORES, E, ELOC, DFF = cfg["NCORES"], cfg["E"], cfg["ELOC"], cfg["DFF"]
    f = lambda a: np.ascontiguousarray(np.asarray(a, dtype=np.float32))
    maps = []
    for c in range(NCORES):
        m = {
            "x": f(inputs["x"][c]),
            "ln_emb_w": f(inputs["ln_emb_w"]).reshape(1, D), "ln_emb_b": f(inputs["ln_emb_b"]).reshape(1, D),
            "w_in": f(inputs["w_in"][0]), "b_qkv": f(inputs["b_qkv"]).reshape(1, 1280),
            "lb_logits": f(inputs["lb_logits"]), "hg_norm_w": f(inputs["hg_norm_w"]).reshape(1, 128),
            "attn_sinks": f(inputs["attn_sinks"]).reshape(1, 16), "attn_norm_w": f(inputs["attn_norm_w"]).reshape(1, 1024),
            "w_out": f(inputs["w_out"][0]), "b_out": f(inputs["b_out"]).reshape(1, D),
            "ln1_w": f(inputs["ln1_w"]).reshape(1, D), "ln1_b": f(inputs["ln1_b"]).reshape(1, D),
            "w_router": f(inputs["w_router"][0]), "b_router": f(inputs["b_router"]).reshape(1, E),
            "b_gate": f(inputs["b_gate"][0]).reshape(E * DFF // 128, 128), "b_up": f(inputs["b_up"][0]).reshape(E * DFF // 128, 128),
            "b_down": f(inputs["b_down"][0]).reshape(E, D),
            "ln2_w": f(inputs["ln2_w"]).reshape(1, D), "ln2_b": f(inputs["ln2_b"]).reshape(1, D),
        }
        NP = cfg.get("NP", 1)
        EP = E // NP
        for p in range(NP):
            m[f"w_gate{p}"] = f(inputs["w_gate"][0][p * EP:(p + 1) * EP]).reshape(EP * D, DFF)
            m[f"w_up{p}"] = f(inputs["w_up"][0][p * EP:(p + 1) * EP]).reshape(EP * D, DFF)
            m[f"w_down{p}"] = f(inputs["w_down"][0][p * EP:(p + 1) * EP]).reshape(EP * DFF, D)
        maps.append(m)
    return maps


def kernel(**inputs):
    cfg = FULL_CFG
    nc = build(cfg)
    maps = make_in_maps(cfg, inputs)
    res = run_bass_kernel_spmd(nc, maps, core_ids=list(range(cfg["NCORES"])))
    return np.stack([np.asarray(r["out"], dtype=np.float32) for r in res.results], axis=0)
```

```python
import contextlib
import numpy as np
import concourse.bass as bass
import concourse.mybir as mybir
from concourse.bass_utils import run_bass_kernel_spmd

F32 = mybir.dt.float32
BF16 = mybir.dt.bfloat16
I32 = mybir.dt.int32
U32 = mybir.dt.uint32
AF = mybir.ActivationFunctionType
ALU = mybir.AluOpType
AX = mybir.AxisListType

D = 2048
KD = 16
ALPHA = 2.0 ** 0.25
EPS = 1e-5
NEG = -30000.0


class Buf:
    __slots__ = ("w", "r")

    def __init__(self):
        self.w = None
        self.r = {}


class Sched:
    ENGS = ("pe", "act", "dve", "pool", "sp")
    K = 8

    def __init__(self, nc, st, window=8):
        self.nc, self.st, self.window = nc, st, window
        self.prog = {e: [] for e in self.ENGS}
        self.sem, self.cnt, self.nsem = {}, {}, 0
        self.idx = {e: 0 for e in self.ENGS}
        self.seen = {e: {} for e in self.ENGS}
        self.dq = {}
        self.ndma = 0
        for e in ("pe", "act", "dve", "pool"):
            self._rot(e)

    def _newsem(self, nm):
        self.nsem += 1
        return self.st.enter_context(self.nc.semaphore(f"{nm}_{self.nsem}"))

    def _rot(self, e):
        self.sem[e] = self._newsem("s" + e)
        self.cnt[e] = 0

    def _deps(self, reads, writes):
        deps = []
        for b in reads:
            deps.append(b.w)
        for b in writes:
            deps.append(b.w)
            deps.extend(b.r.values())
        return deps

    def _waits(self, eng, deps):
        best = {}
        for t in deps:
            if t is None:
                continue
            sem, val, teng, tidx = t
            if teng == eng:
                if eng == "pe" or self.idx[eng] - tidx > self.window:
                    continue
            k = id(sem)
            if self.seen[eng].get(k, 0) >= val:
                continue
            if k not in best or best[k][1] < val:
                best[k] = (sem, val)
        out = []
        for k, (sem, val) in best.items():
            self.seen[eng][k] = val
            out.append((sem, val))
        return out

    def op(self, eng, meth, reads=(), writes=(), **kw):
        waits = self._waits(eng, self._deps(reads, writes))
        self.cnt[eng] += 1
        self.idx[eng] += 1
        tk = (self.sem[eng], self.cnt[eng], eng, self.idx[eng])
        self.prog[eng].append((waits, meth, kw, self.sem[eng], 1))
        if self.cnt[eng] >= 30000:
            self._rot(eng)
        for b in reads:
            b.r[eng] = tk
        for b in writes:
            b.w = tk
            b.r = {}
        return tk

    def dma(self, q, reads=(), writes=(), meth="dma_start", **kw):
        d = self.dq.get(q)
        if d is None:
            d = self.dq[q] = {"sems": [self._newsem("d" + q) for _ in range(self.K)], "cnt": [0] * self.K, "n": 0}
        i = d["n"] % self.K
        d["n"] += 1
        self.ndma += 1
        sem, prev = d["sems"][i], d["cnt"][i]
        deps = self._deps(reads, writes)
        if prev > 0:
            deps.append((sem, prev, None, 0))
        waits = self._waits(q, deps)
        d["cnt"][i] = prev + 16
        tk = (sem, prev + 16, None, 0)
        self.prog[q].append((waits, meth, kw, sem, 16))
        for b in reads:
            b.r[("dma", self.ndma)] = tk
        for b in writes:
            b.w = tk
            b.r = {}
        return tk

    def special(self, q, buf, meth, **kw):
        sem = self._newsem("x" + q)
        self.prog[q].append(([], meth, kw, sem, 16))
        buf.w = (sem, 16, None, 0)
        buf.r = {}

    def barrier(self):
        last = []
        for e in ("pe", "act", "dve", "pool"):
            if self.cnt[e] > 0:
                last.append((self.sem[e], self.cnt[e], e, self.idx[e]))
        for q, d in self.dq.items():
            for sem, c in zip(d["sems"], d["cnt"]):
                if c > 0:
                    last.append((sem, c, None, 0))
        for e in self.ENGS:
            deps = [t for t in last if t[2] != e]
            waits = self._waits(e, deps)
            if waits:
                self.prog[e].append((waits, None, None, None, 0))

    def wait_all(self, eng, tickets):
        waits = self._waits(eng, list(tickets))
        self.prog[eng].append((waits, None, None, None, 0))

    def emit(self, block):
        m = {"pe": block.tensor, "act": block.scalar, "dve": block.vector, "pool": block.gpsimd, "sp": block.sync}
        for e in self.ENGS:
            prog = self.prog[e]

            def body(eng, prog=prog):
                for waits, meth, kw, sem, inc in prog:
                    for s, v in waits:
                        eng.wait_ge(s, v)
                    if meth is not None:
                        try:
                            getattr(eng, meth)(**kw).then_inc(sem, inc)
                        except Exception:
                            print("FAILED OP", meth, {k: str(v)[:400] for k, v in kw.items()})
                            if meth == "indirect_dma_start":
                                print("IDX AP", kw["out_offset"].ap if kw.get("out_offset") is not None else None)
                                for drop in (("bounds_check", "oob_is_err"),):
                                    kw2 = {k: v for k, v in kw.items() if k not in drop}
                                    try:
                                        getattr(eng, meth)(**kw2)
                                        print("WORKS without", drop)
                                    except Exception as ex2:
                                        print("still fails without", drop, repr(ex2)[:200])
                            raise

            m[e](body)


def build(cfg):
    NCORES, E, ELOC, DFF, C, SEQ = cfg["NCORES"], cfg["E"], cfg["ELOC"], cfg["DFF"], cfg["C"], cfg["SEQ"]
    STOP = cfg.get("STOP", "full")
    NT = SEQ // 128
    KF = DFF // 128
    CT = C // 128
    TB = 512
    NTB = SEQ // TB
    nc = bass.Bass("TRN2", target_bir_lowering=False)

    def din(name, shape, dt=F32):
        return nc.dram_tensor(name, shape, dt, kind="ExternalInput").ap()

    def dscr(name, shape, dt):
        return nc.dram_tensor(name, shape, dt, kind="Internal").ap()

    x = din("x", [SEQ, D])
    ln_emb_w, ln_emb_b = din("ln_emb_w", [1, D]), din("ln_emb_b", [1, D])
    w_in = din("w_in", [D, 5376])
    b_qkv = din("b_qkv", [1, 1280])
    lb_logits = din("lb_logits", [2, 1024])
    hg_norm_w = din("hg_norm_w", [1, 128])
    attn_sinks = din("attn_sinks", [1, 16])
    attn_norm_w = din("attn_norm_w", [1, 1024])
    w_out = din("w_out", [D, D])
    b_out = din("b_out", [1, D])
    ln1_w, ln1_b = din("ln1_w", [1, D]), din("ln1_b", [1, D])
    w_router = din("w_router", [D, E])
    b_router = din("b_router", [1, E])
    NP = cfg.get("NP", 1)
    EP = E // NP
    w_gate = [din(f"w_gate{p}", [EP * D, DFF]) for p in range(NP)]
    w_up = [din(f"w_up{p}", [EP * D, DFF]) for p in range(NP)]
    w_down = [din(f"w_down{p}", [EP * DFF, D]) for p in range(NP)]
    b_gate = din("b_gate", [E * KF, 128])
    b_up = din("b_up", [E * KF, 128])
    b_down = din("b_down", [E, D])
    ln2_w, ln2_b = din("ln2_w", [1, D]), din("ln2_b", [1, D])
    out = nc.dram_tensor("out", [SEQ, D], F32, kind="ExternalOutput").ap()
    dbg = None
    if STOP != "full":
        dbg = nc.dram_tensor("dbg", [SEQ, D], F32, kind="ExternalOutput").ap()

    h0_d = dscr("h0_d", [SEQ, D], F32)
    mix_d = dscr("mix_d", [SEQ, D], BF16)
    h1_d = dscr("h1_d", [SEQ, D], F32)
    h1b_d = dscr("h1b_d", [SEQ + 128, D], BF16)
    slot_d = dscr("slot_d", [E * C + 128, 16], I32)
    yall_d = dscr("yall_d", [E * C + 128, D], F32)
    with contextlib.ExitStack() as st:
        S = Sched(nc, st)

        def sb(name, shape, dt):
            return st.enter_context(nc.sbuf_tensor(name, shape, dt))

        ARENA_W = 43008
        arena = sb("arena", [128, ARENA_W], F32)
        atop = [0]

        def al(name, shape, dt):
            esz = 4 if dt in (F32, I32, U32) else 2
            n = 1
            for d_ in shape[1:]:
                n *= d_
            words = (n * esz + 3) // 4
            words = (words + 7) // 8 * 8
            a0 = atop[0]
            atop[0] += words
            assert atop[0] <= ARENA_W, (name, atop[0])
            v = arena[:, a0:a0 + words]
            if dt != F32:
                v = v.bitcast(dt)
            v = v[:, 0:n]
            if len(shape) == 3:
                v = v.rearrange("p (a b) -> p a b", a=shape[1])
            return v

        def release(mark):
            S.barrier()
            atop[0] = mark

        PS = [st.enter_context(nc.psum_tensor(f"ps{i}", [128, 512], F32)) for i in range(8)]
        PSB = [Buf() for _ in range(8)]
        psn = [0]

        def ps_get(n=1):
            if (psn[0] % 8) + n > 8:
                psn[0] += 8 - (psn[0] % 8)
            i = psn[0] % 8
            psn[0] += n
            return list(range(i, i + n))

        ident_f = sb("ident_f", [128, 128], F32)
        ident_b = sb("ident_b", [128, 128], BF16)
        ones_b = sb("ones_b", [128, 128], BF16)
        lstrict = sb("lstrict", [128, 128], BF16)
        hmask = sb("hmask", [128, 128], F32)
        amask = sb("amask", [128, 256], F32)
        m01 = sb("m01", [128, TB], F32)
        iota_e = sb("iota_e", [128, E], F32)
        rowbase = sb("rowbase", [128, E], F32)
        tokid = sb("tokid", [128, NT * 16], I32)
        padidx = sb("padidx", [128, 1], I32)
        zer_b = sb("zer_b", [128, D], BF16)
        CB = Buf()
        P = lambda meth, **kw: S.op("pool", meth, (), (CB,), **kw)
        P("memset", ap=ident_f[:], constant=1.0)
        P("affine_select", out=ident_f[:], in_=ident_f[:], pattern=[[-1, 128]], compare_op=ALU.is_equal, fill=0.0, base=0, channel_multiplier=1)
        P("tensor_copy", out=ident_b[:], in_=ident_f[:])
        P("memset", ap=ones_b[:], constant=1.0)
        P("memset", ap=hmask[:], constant=1.0)
        P("affine_select", out=hmask[:], in_=hmask[:], pattern=[[1, 128]], compare_op=ALU.is_ge, fill=0.0, base=-1, channel_multiplier=-1)
        P("tensor_copy", out=lstrict[:], in_=hmask[:])
        P("memset", ap=hmask[:], constant=1.0)
        P("affine_select", out=hmask[:], in_=hmask[:], pattern=[[1, 128]], compare_op=ALU.is_ge, fill=0.0, base=0, channel_multiplier=-1)
        P("memset", ap=hmask[0:64, 64:128], constant=0.0)
        P("memset", ap=amask[:], constant=0.0)
        P("affine_select", out=amask[:], in_=amask[:], pattern=[[1, 256]], compare_op=ALU.is_ge, fill=NEG, base=-1, channel_multiplier=-1)
        P("affine_select", out=amask[:], in_=amask[:], pattern=[[-1, 256]], compare_op=ALU.is_ge, fill=NEG, base=128, channel_multiplier=1)
        P("memset", ap=m01[:], constant=1.0)
        P("memset", ap=m01[:].rearrange("p (c t) -> p c t", t=64)[:, :, 0:1], constant=0.0)
        P("iota", out=iota_e[:], pattern=[[1, E]], base=0, channel_multiplier=0, allow_small_or_imprecise_dtypes=True)
        P("iota", out=rowbase[:], pattern=[[C, E]], base=0, channel_multiplier=0, allow_small_or_imprecise_dtypes=True)
        P("iota", out=tokid[:], pattern=[[128, NT], [0, 16]], base=0, channel_multiplier=1)
        P("memset", ap=padidx[:], constant=SEQ)
        P("memset", ap=zer_b[:], constant=0.0)

        pw = sb("pw", [128, D], F32)
        pb = sb("pb", [128, D], F32)
        PWB, PBB = Buf(), Buf()

        def load_bcast(dst, dbuf, src_row, n):
            S.dma("sp", (), (dbuf,), out=dst[:, 0:n], in_=src_row.partition_broadcast(128))

        stats = sb("stats", [128, 4, 6], F32)
        mv = sb("mv", [128, 2], F32)
        sm1 = sb("sm1", [128, 8], F32)
        STB = Buf()

        def layer_norm(src, sbuf, dst, dbuf, w_t, w_b, b_t, b_b):
            for c in range(4):
                S.op("dve", "bn_stats", (sbuf,), (STB,), out=stats[:, c, :], in_=src[:, c * 512:(c + 1) * 512])
            S.op("dve", "bn_aggr", (STB,), (STB,), out=mv[:], in_=stats[:].rearrange("p a b -> p (a b)"))
            S.op("dve", "tensor_scalar", (STB,), (STB,), out=sm1[:, 0:1], in0=mv[:, 1:2], scalar1=EPS, scalar2=None, op0=ALU.add)
            S.op("act", "activation", (STB,), (STB,), out=sm1[:, 1:2], in_=sm1[:, 0:1], func=AF.Sqrt)
            S.op("dve", "reciprocal", (STB,), (STB,), out=sm1[:, 2:3], in_=sm1[:, 1:2])
            S.op("dve", "scalar_tensor_tensor", (STB,), (STB,), out=sm1[:, 3:4], in0=mv[:, 0:1], scalar=-1.0, in1=sm1[:, 2:3], op0=ALU.mult, op1=ALU.mult)
            S.op("act", "activation", (sbuf, STB), (dbuf,), out=dst[:], in_=src[:], func=AF.Identity, bias=sm1[:, 3:4], scale=sm1[:, 2:3])
            S.op("dve", "tensor_tensor", (dbuf, w_b), (dbuf,), out=dst[:], in0=dst[:], in1=w_t[:], op=ALU.mult)
            S.op("pool", "tensor_tensor", (dbuf, b_b), (dbuf,), out=dst[:], in0=dst[:], in1=b_t[:], op=ALU.add)

        hT = al("hT", [128, KD, SEQ], BF16)
        mark_hT = atop[0]
        HTB = [Buf() for _ in range(NT)]
        tA = [al(f"tA{i}", [128, D], F32) for i in range(3)]
        tAB = [Buf(), Buf(), Buf()]
        tB = [al(f"tB{i}", [128, D], F32) for i in range(3)]
        tBB = [Buf(), Buf(), Buf()]
        tC = [al(f"tC{i}", [128, D], BF16) for i in range(3)]
        tCB = [Buf(), Buf(), Buf()]

        out_tk = []

        WINB, WOUTB = Buf(), Buf()

        load_bcast(pw, PWB, ln_emb_w[0:1, :], D)
        load_bcast(pb, PBB, ln_emb_b[0:1, :], D)
        H0B = [Buf() for _ in range(NT)]
        for i in range(NT):
            a, ab = tA[i % 3], tAB[i % 3]
            b, bb = tB[i % 3], tBB[i % 3]
            c, cb = tC[i % 3], tCB[i % 3]
            S.dma("sp", (), (ab,), out=a[:], in_=x[i * 128:(i + 1) * 128, :])
            layer_norm(a, ab, b, bb, pw, PWB, pb, PBB)
            S.dma("sp", (bb,), (H0B[i],), out=h0_d[i * 128:(i + 1) * 128, :], in_=b[:])
            S.op("act", "activation", (bb,), (cb,), out=c[:], in_=b[:], func=AF.Copy)
            for g in range(4):
                (pi,) = ps_get()
                pv = PS[pi][:].bitcast(BF16)
                for j in range(4):
                    kd = g * 4 + j
                    S.op("pe", "transpose", (cb,), (PSB[pi],), out=pv[:, j * 128:(j + 1) * 128], in_=c[:, kd * 128:(kd + 1) * 128], identity=ident_b[:])
                S.op("dve" if g % 2 == 0 else "act", "tensor_copy" if g % 2 == 0 else "copy", (PSB[pi],), (HTB[i],),
                     out=hT[:, g * 4:(g + 1) * 4, i * 128:(i + 1) * 128],
                     in_=pv[:, 0:512].rearrange("p (j t) -> p j t", j=4))
        if STOP == "ln":
            for i in range(NT):
                out_tk.append(S.dma("sp", (H0B[i],), (), out=dbg[i * 128:(i + 1) * 128, :], in_=h0_d[i * 128:(i + 1) * 128, :]))

        release(mark_hT)
        lbt = sb("lbt", [128, 8, 4], F32)
        LBB = Buf()
        if STOP not in ("ln",):
            S.dma("sp", (), (LBB,), out=lbt[:, :, 0:1], in_=lb_logits[0:1, :].rearrange("o (h p) -> p h o", p=128), allow_slow_non_contiguous=True)
            S.dma("sp", (), (LBB,), out=lbt[:, :, 1:2], in_=lb_logits[1:2, :].rearrange("o (h p) -> p h o", p=128), allow_slow_non_contiguous=True)
            S.op("dve", "tensor_tensor", (LBB,), (LBB,), out=lbt[:, :, 2:3], in0=lbt[:, :, 0:1], in1=lbt[:, :, 1:2], op=ALU.subtract)
            S.op("act", "activation", (LBB,), (LBB,), out=lbt[:, :, 2:3], in_=lbt[:, :, 2:3], func=AF.Sigmoid)
            S.op("dve", "tensor_scalar", (LBB,), (LBB,), out=lbt[:, :, 3:4], in0=lbt[:, :, 2:3], scalar1=-1.0, scalar2=1.0, op0=ALU.mult, op1=ALU.add)
            hgw = sb("hgw", [128, 128], F32)
            HGWB = Buf()
            S.dma("sp", (), (HGWB,), out=hgw[:], in_=hg_norm_w[0:1, :].partition_broadcast(128))

            wh = [al(f"wh{i}", [128, KD, 512], BF16) for i in range(2)]
            WHB = [Buf(), Buf()]
            QT = al("QT", [128, SEQ], BF16)
            KT = al("KT", [128, SEQ], BF16)
            QTB, KTB = Buf(), Buf()
            QTa = al("QTa", [128, SEQ], BF16)
            QTb = al("QTb", [128, SEQ], BF16)
            QTaB, QTbB = Buf(), Buf()
            Vt0 = al("Vt0", [128, NT, 128], BF16)
            Vt1 = al("Vt1", [128, NT, 128], BF16)
            Vt0B, Vt1B = Buf(), Buf()
            S.op("pool", "memset", (), (QTaB,), ap=QTa[:], constant=0.0)
            S.op("pool", "memset", (), (QTbB,), ap=QTb[:], constant=0.0)
            S.op("pool", "memset", (), (Vt0B,), ap=Vt0[:], constant=0.0)
            S.op("pool", "memset", (), (Vt1B,), ap=Vt1[:], constant=0.0)
            EBL = al("EBL", [128, SEQ // 64], F32)
            EBLB = Buf()
            Ktm = al("Ktm", [128, NT, 128], BF16)
            Vtm = al("Vtm", [128, NT, 128], BF16)
            GW = al("GW", [128, NT, 128], F32)
            Yh = al("Yh", [128, NT, 128], BF16)
            KtmB, VtmB, GWB, YhB = Buf(), Buf(), Buf(), Buf()
            ft = [al(f"ft{i}", [128, TB], F32) for i in range(6)]
            FTB = [Buf() for _ in range(6)]
            Sf = [al(f"Sf{i}", [128, 128], F32) for i in range(2)]
            Sb_ = [al(f"Sb{i}", [128, 128], BF16) for i in range(2)]
            SfB = [Buf(), Buf()]
            SbB = [Buf(), Buf()]
            sT = [al(f"sT{i}", [128, 128], BF16) for i in range(2)]
            sTB = [Buf(), Buf()]
            tmpS = al("tmpS", [128, 128], F32)
            tmpSB = Buf()
            hsm = sb("hsm", [128, 8], F32)
            hsm2 = sb("hsm2", [128, 8], F32)
            HSMB2 = Buf()
            HSMB = Buf()
            junk = al("junk", [128, 128], F32)
            JB = Buf()

            NH = cfg.get("NH", 8)
            for hh in range(NH):
                w, wb = wh[hh % 2], WHB[hh % 2]
                for gi, base in enumerate((0, 1024, 2048, 3072)):
                    S.dma("pool", (WINB,), (wb,), out=w[:, :, gi * 128:(gi + 1) * 128],
                          in_=w_in[:, base + hh * 128: base + (hh + 1) * 128].rearrange("(k p) c -> p k c", p=128))
                for tb in range(NTB):
                    tok = slice(tb * TB, (tb + 1) * TB)
                    pq, pf = ps_get(2)
                    for kd in range(KD):
                        S.op("pe", "matmul", (wb,) + tuple(HTB[tb * 4:(tb + 1) * 4]), (PSB[pq],), out=PS[pq][:], lhsT=w[:, kd, 0:128], rhs=hT[:, kd, tok], start=(kd == 0), stop=(kd == KD - 1))
                    for kd in range(KD):
                        S.op("pe", "matmul", (wb,) + tuple(HTB[tb * 4:(tb + 1) * 4]), (PSB[pf],), out=PS[pf][:], lhsT=w[:, kd, 128:256], rhs=hT[:, kd, tok], start=(kd == 0), stop=(kd == KD - 1))
                    sig, f, lf, bb_, eb, enb = ft
                    S.op("act", "activation", (PSB[pf],), (FTB[0],), out=sig[:], in_=PS[pf][:], func=AF.Sigmoid)
                    S.op("dve", "tensor_scalar", (FTB[0], LBB), (FTB[1],), out=f[:], in0=sig[:], scalar1=lbt[:, hh, 3:4], scalar2=lbt[:, hh, 2:3], op0=ALU.mult, op1=ALU.add)
                    S.op("act", "activation", (FTB[1],), (FTB[2],), out=lf[:], in_=f[:], func=AF.Ln)
                    S.op("dve", "tensor_tensor_scan", (FTB[2], CB), (FTB[3],), out=bb_[:], data0=m01[:], data1=lf[:], initial=0.0, op0=ALU.mult, op1=ALU.add)
                    S.op("act", "activation", (FTB[3],), (FTB[4],), out=eb[:], in_=bb_[:], func=AF.Exp)
                    S.op("act", "activation", (FTB[3],), (FTB[5],), out=enb[:], in_=bb_[:], func=AF.Exp, scale=-1.0)
                    S.op("pool", "tensor_copy", (FTB[4],), (EBLB,), out=EBL[:, tb * 8:(tb + 1) * 8], in_=eb[:].rearrange("p (c t) -> p c t", t=64)[:, :, 63])
                    S.op("act", "activation", (PSB[pq],), (FTB[0],), out=sig[:], in_=PS[pq][:], func=AF.Sigmoid)
                    S.op("dve", "tensor_tensor", (FTB[0], PSB[pq]), (FTB[0],), out=sig[:], in0=sig[:], in1=PS[pq][:], op=ALU.mult)
                    S.op("dve", "tensor_tensor", (FTB[0], FTB[4]), (QTB,), out=QT[:, tok], in0=sig[:], in1=eb[:], op=ALU.mult)
                    qv = lambda t_: t_[:, tok].rearrange("p (c two t) -> p c two t", two=2, t=64)
                    S.op("pool", "tensor_copy", (QTB,), (QTaB,), out=qv(QTa)[:, :, 0, :], in_=qv(QT)[:, :, 0, :])
                    S.op("pool", "tensor_copy", (QTB,), (QTbB,), out=qv(QTb)[:, :, 1, :], in_=qv(QT)[:, :, 1, :])
                    S.op("pool", "tensor_scalar", (FTB[1],), (FTB[2],), out=lf[:], in0=f[:], scalar1=-1.0, scalar2=1.0, op0=ALU.mult, op1=ALU.add)
                    S.op("pool", "tensor_tensor", (FTB[2], FTB[5]), (KTB,), out=KT[:, tok], in0=lf[:], in1=enb[:], op=ALU.mult)
                HS = cfg.get("HSTOP", 3)
                for i in range(NT if HS >= 2 else 0):
                    (pv_,) = ps_get()
                    for kd in range(KD):
                        S.op("pe", "matmul", (wb, HTB[i]), (PSB[pv_],), out=PS[pv_][:, 0:256], lhsT=hT[:, kd, i * 128:(i + 1) * 128], rhs=w[:, kd, 256:512], start=(kd == 0), stop=(kd == KD - 1))
                    S.op("act", "copy", (PSB[pv_],), (VtmB,), out=Vtm[:, i, :], in_=PS[pv_][:, 0:128])
                    S.op("act", "copy", (PSB[pv_],), (Vt0B,), out=Vt0[0:64, i, :], in_=PS[pv_][0:64, 0:128])
                    S.op("act", "copy", (PSB[pv_],), (Vt1B,), out=Vt1[64:128, i, :], in_=PS[pv_][64:128, 0:128])
                    S.op("act", "activation", (PSB[pv_],), (JB,), out=junk[:], in_=PS[pv_][:, 128:256], func=AF.Sigmoid)
                    S.op("dve", "tensor_tensor", (JB, PSB[pv_]), (JB,), out=junk[:], in0=junk[:], in1=PS[pv_][:, 128:256], op=ALU.mult)
                    S.op("pool", "tensor_tensor", (JB, HGWB), (GWB,), out=GW[:, i, :], in0=junk[:], in1=hgw[:], op=ALU.mult)
                    (pt,) = ps_get()
                    ptv = PS[pt][:].bitcast(BF16)
                    S.op("pe", "transpose", (KTB,), (PSB[pt],), out=ptv[:, 0:128], in_=KT[:, i * 128:(i + 1) * 128], identity=ident_b[:])
                    S.op("dve", "tensor_copy", (PSB[pt],), (KtmB,), out=Ktm[:, i, :], in_=ptv[:, 0:128])
                S.op("pool", "memset", (), (SfB[0],), ap=Sf[0][:], constant=0.0)
                S.op("pool", "memset", (), (SbB[0],), ap=Sb_[0][:], constant=0.0)
                hsms = [hsm, hsm2]
                hsmbs = [HSMB, HSMB2]
                stA = {}

                def stage_a(i):
                    tk_ = slice(i * 128, (i + 1) * 128)
                    (psc,) = ps_get()
                    S.op("pe", "matmul", (KTB, QTB), (PSB[psc],), out=PS[psc][:, 0:128], lhsT=KT[:, tk_], rhs=QT[:, tk_], start=True, stop=True)
                    s_, s_b = sT[i % 2], sTB[i % 2]
                    S.op("dve", "tensor_tensor", (PSB[psc], CB), (s_b,), out=s_[:], in0=PS[psc][:, 0:128], in1=hmask[:], op=ALU.mult)
                    pps = []
                    for c in range(2):
                        (pp,) = ps_get()
                        S.op("pe", "matmul", (KtmB, Vt0B, Vt1B), (PSB[pp],), out=PS[pp][:, 0:128], lhsT=Ktm[:, i, :], rhs=(Vt0, Vt1)[c][:, i, :], start=True, stop=True)
                        pps.append(pp)
                    stA[i] = pps

                def stage_b(i, cur):
                    tk_ = slice(i * 128, (i + 1) * 128)
                    s_, s_b = sT[i % 2], sTB[i % 2]
                    pps = stA.pop(i)
                    (po,) = ps_get()
                    S.op("pe", "matmul", (s_b, VtmB), (PSB[po],), out=PS[po][:, 0:128], lhsT=s_[:], rhs=Vtm[:, i, :], start=True, stop=False)
                    for c in range(2):
                        S.op("pe", "matmul", (QTaB, QTbB, SbB[cur]), (PSB[po],), out=PS[po][:, 0:128], lhsT=(QTa, QTb)[c][:, tk_], rhs=Sb_[cur][:], start=False, stop=(c == 1))
                        pp = pps[c]
                        nxt = 1 - cur
                        S.op("dve", "tensor_tensor", (PSB[pp], SfB[cur]), (tmpSB,), out=tmpS[:], in0=PS[pp][:, 0:128], in1=Sf[cur][:], op=ALU.add)
                        S.op("dve", "tensor_scalar", (tmpSB, EBLB), (SfB[nxt],), out=Sf[nxt][:], in0=tmpS[:], scalar1=EBL[:, 2 * i + c: 2 * i + c + 1], scalar2=None, op0=ALU.mult)
                        S.op("act", "copy", (SfB[nxt],), (SbB[nxt],), out=Sb_[nxt][:], in_=Sf[nxt][:])
                        cur = nxt
                    hs_, hb_ = hsms[i % 2], hsmbs[i % 2]
                    S.op("act", "activation", (PSB[po],), (JB, hb_), out=junk[:], in_=PS[po][:, 0:128], func=AF.Square, accum_out=hs_[:, 0:1])
                    S.op("dve", "tensor_scalar", (hb_,), (hb_,), out=hs_[:, 1:2], in0=hs_[:, 0:1], scalar1=1.0 / 128, scalar2=EPS, op0=ALU.mult, op1=ALU.add)
                    S.op("act", "activation", (hb_,), (hb_,), out=hs_[:, 2:3], in_=hs_[:, 1:2], func=AF.Sqrt)
                    S.op("dve", "reciprocal", (hb_,), (hb_,), out=hs_[:, 3:4], in_=hs_[:, 2:3])
                    S.op("dve", "scalar_tensor_tensor", (PSB[po], hb_, GWB), (YhB,), out=Yh[:, i, :], in0=PS[po][:, 0:128], scalar=hs_[:, 3:4], in1=GW[:, i, :], op0=ALU.mult, op1=ALU.mult)
                    return cur

                cur = 0
                if HS >= 3:
                    stage_a(0)
                    for i in range(NT):
                        if i + 1 < NT:
                            stage_a(i + 1)
                        cur = stage_b(i, cur)
                S.dma("sp", (YhB,), (), out=mix_d[:, hh * 128:(hh + 1) * 128].rearrange("(i p) v -> p i v", p=128), in_=Yh[:])
            if STOP == "hgrn":
                S.op("pool", "tensor_copy", (YhB,), (tAB[0],), out=tA[0][:, 0:NT * 128].rearrange("p (i v) -> p i v", v=128), in_=Yh[:])
                out_tk.append(S.dma("sp", (tAB[0],), (), out=dbg[0:128, 0:NT * 128], in_=tA[0][:, 0:NT * 128]))

        IOA = bass.IndirectOffsetOnAxis
        if STOP not in ("ln", "hgrn"):
            release(mark_hT)
            wq = [al(f"wq{i}", [128, KD, 128], BF16) for i in range(2)]
            WQB = [Buf(), Buf()]
            wkv = al("wkv", [128, KD, 256], BF16)
            WKVB = Buf()
            qT = al("qT", [128, 8, SEQ], BF16)
            QB = [Buf() for _ in range(8)]
            kTa = al("kTa", [128, SEQ], BF16)
            kTb = al("kTb", [128, SEQ], BF16)
            KAB, KBB = Buf(), Buf()
            Vs = al("Vs", [128, NT, 128], BF16)
            VSB = Buf()
            bqt = al("bqt", [128, 8, 1], F32)
            bkt = al("bkt", [128, 1, 1], F32)
            bvb = al("bvb", [128, 128], F32)
            sink_t = al("sink_t", [128, 16], F32)
            anw = al("anw", [128, 1024], F32)
            PRB = Buf()
            S.dma("sp", (), (PRB,), out=bqt[0:64, :, :], in_=b_qkv[0:1, 0:512].rearrange("o (j p) -> p j o", p=64), allow_slow_non_contiguous=True)
            S.dma("sp", (), (PRB,), out=bqt[64:128, :, :], in_=b_qkv[0:1, 512:1024].rearrange("o (j p) -> p j o", p=64), allow_slow_non_contiguous=True)
            S.dma("sp", (), (PRB,), out=bkt[:, :, :], in_=b_qkv[0:1, 1024:1152].rearrange("o (j p) -> p j o", p=128), allow_slow_non_contiguous=True)
            S.dma("sp", (), (PRB,), out=bvb[:, :], in_=b_qkv[0:1, 1152:1280].partition_broadcast(128))
            S.dma("sp", (), (PRB,), out=sink_t[:, :], in_=attn_sinks[0:1, :].partition_broadcast(128))
            S.dma("sp", (), (PRB,), out=anw[:, :], in_=attn_norm_w[0:1, :].partition_broadcast(128))
            S.op("dve", "tensor_scalar", (PRB,), (PRB,), out=bqt[:, :, 0], in0=bqt[:, :, 0], scalar1=0.125, scalar2=None, op0=ALU.mult)
            S.op("pool", "memset", (), (KAB,), ap=kTa[:], constant=0.0)
            S.op("pool", "memset", (), (KBB,), ap=kTb[:], constant=0.0)
            for half, c0 in ((0, 5120), (1, 5248)):
                S.dma("pool", (WINB,), (WKVB,), out=wkv[:, :, half * 128:(half + 1) * 128], in_=w_in[:, c0:c0 + 128].rearrange("(k p) c -> p k c", p=128))
            allh = tuple(HTB)
            for tb in range(NTB):
                tok = slice(tb * TB, (tb + 1) * TB)
                (pk,) = ps_get()
                for kd in range(KD):
                    S.op("pe", "matmul", (WKVB,) + allh, (PSB[pk],), out=PS[pk][:], lhsT=wkv[:, kd, 0:128], rhs=hT[:, kd, tok], start=(kd == 0), stop=(kd == KD - 1))
                S.op("act", "activation", (PSB[pk], PRB), (KAB,), out=kTa[0:64, tok], in_=PS[pk][0:64, :], func=AF.Identity, bias=bkt[0:64, 0, :], scale=1.0)
                S.op("act", "activation", (PSB[pk], PRB), (KBB,), out=kTb[64:128, tok], in_=PS[pk][64:128, :], func=AF.Identity, bias=bkt[64:128, 0, :], scale=1.0)
            for i in range(NT):
                (pv_,) = ps_get()
                for kd in range(KD):
                    S.op("pe", "matmul", (WKVB,) + allh, (PSB[pv_],), out=PS[pv_][:, 0:128], lhsT=hT[:, kd, i * 128:(i + 1) * 128], rhs=wkv[:, kd, 128:256], start=(kd == 0), stop=(kd == KD - 1))
                S.op("dve", "tensor_tensor", (PSB[pv_], PRB), (VSB,), out=Vs[:, i, :], in0=PS[pv_][:, 0:128], in1=bvb[:], op=ALU.add)
            for j in range(8):
                w, wb = wq[j % 2], WQB[j % 2]
                for half, hd in ((0, j), (1, 8 + j)):
                    S.dma("pool", (WINB,), (wb,), out=w[:, :, half * 64:(half + 1) * 64], in_=w_in[:, 4096 + hd * 64: 4096 + (hd + 1) * 64].rearrange("(k p) c -> p k c", p=128))
                for tb in range(NTB):
                    tok = slice(tb * TB, (tb + 1) * TB)
                    (pq,) = ps_get()
                    for kd in range(KD):
                        S.op("pe", "matmul", (wb,) + allh, (PSB[pq],), out=PS[pq][:], lhsT=w[:, kd, :], rhs=hT[:, kd, tok], start=(kd == 0), stop=(kd == KD - 1))
                    S.op("act", "activation", (PSB[pq], PRB), (QB[j],), out=qT[:, j, tok], in_=PS[pq][:], func=AF.Identity, bias=bqt[:, j, :], scale=0.125)
            smx = [al(f"smx{i}", [128, 256], F32) for i in range(4)]
            SMB = [Buf() for _ in range(4)]
            ex = [al(f"ex{i}", [128, 256], BF16) for i in range(4)]
            EXB = [Buf() for _ in range(4)]
            eT = [al(f"eT{i}", [128, 256], BF16) for i in range(4)]
            ETB = [Buf() for _ in range(4)]
            stt = [al(f"stt{i}", [128, 8], F32) for i in range(4)]
            STTB = [Buf() for _ in range(4)]
            osw = [al(f"osw{i}", [128, 1024], F32) for i in range(2)]
            OSB = [Buf(), Buf()]
            ysw = [al(f"ysw{i}", [128, 1024], BF16) for i in range(2)]
            YSB = [Buf(), Buf()]
            junk2 = al("junk2", [128, 1024], F32)
            J2B = Buf()
            stt2 = al("stt2", [128, 8], F32)
            STT2B = Buf()
            def swa_iter(n, j, c, it):
                ob, obb = osw[n % 2], OSB[n % 2]
                head = j + 8 * c
                kx, kxb = ((kTa, KAB), (kTb, KBB))[c]
                if n == 0:
                    N_, keys, msk, vblk = 128, slice(0, 128), amask[:, 128:256], [0]
                else:
                    N_, keys, msk, vblk = 256, slice((n - 1) * 128, (n + 1) * 128), amask[:, 0:256], [n - 1, n]
                sm_, smb = smx[it % 4], SMB[it % 4]
                e_, eb_ = ex[it % 4], EXB[it % 4]
                t_, tb_ = eT[it % 4], ETB[it % 4]
                s4, s4b = stt[it % 4], STTB[it % 4]
                loc = {}

                def s0():
                    (psc,) = ps_get()
                    loc["psc"] = psc
                    S.op("pe", "matmul", (QB[j], kxb), (PSB[psc],), out=PS[psc][:, 0:N_], lhsT=qT[:, j, n * 128:(n + 1) * 128], rhs=kx[:, keys], start=True, stop=True)

                def s1():
                    psc = loc["psc"]
                    S.op("dve", "tensor_tensor", (PSB[psc], CB), (smb,), out=sm_[:, 0:N_], in0=PS[psc][:, 0:N_], in1=msk, op=ALU.add)
                    S.op("dve", "reduce_max", (smb,), (s4b,), out=s4[:, 0:1], in_=sm_[:, 0:N_], axis=AX.X)
                    S.op("dve", "tensor_scalar", (s4b,), (s4b,), out=s4[:, 1:2], in0=s4[:, 0:1], scalar1=-1.0, scalar2=None, op0=ALU.mult)

                def s2():
                    S.op("act", "activation", (smb, s4b), (eb_, s4b), out=e_[:, 0:N_], in_=sm_[:, 0:N_], func=AF.Exp, bias=s4[:, 1:2], scale=1.0, accum_out=s4[:, 2:3])
                    S.op("act", "activation", (s4b, PRB), (s4b,), out=s4[:, 3:4], in_=s4[:, 0:1], func=AF.Exp, bias=sink_t[:, head:head + 1], scale=-1.0)

                def s3():
                    (pt,) = ps_get()
                    loc["pt"] = pt
                    ptv = PS[pt][:].bitcast(BF16)
                    for kb in range(N_ // 128):
                        S.op("pe", "transpose", (eb_,), (PSB[pt],), out=ptv[:, kb * 128:(kb + 1) * 128], in_=e_[:, kb * 128:(kb + 1) * 128], identity=ident_b[:])
                    S.op("dve", "tensor_tensor", (s4b,), (s4b,), out=s4[:, 4:5], in0=s4[:, 2:3], in1=s4[:, 3:4], op=ALU.add)
                    S.op("dve", "reciprocal", (s4b,), (s4b,), out=s4[:, 5:6], in_=s4[:, 4:5])

                def s4_():
                    pt = loc["pt"]
                    ptv = PS[pt][:].bitcast(BF16)
                    if it % 2 == 0:
                        S.op("dve", "tensor_copy", (PSB[pt],), (tb_,), out=t_[:, 0:N_], in_=ptv[:, 0:N_])
                    else:
                        S.op("act", "copy", (PSB[pt],), (tb_,), out=t_[:, 0:N_], in_=ptv[:, 0:N_])

                def s5():
                    (po,) = ps_get()
                    for kb, vb in enumerate(vblk):
                        S.op("pe", "matmul", (tb_, VSB), (PSB[po],), out=PS[po][:, 0:64], lhsT=t_[:, kb * 128:(kb + 1) * 128], rhs=Vs[:, vb, c * 64:(c + 1) * 64], start=(kb == 0), stop=(kb == len(vblk) - 1))
                    S.op("act", "activation", (PSB[po], s4b), (obb,), out=ob[:, head * 64:(head + 1) * 64], in_=PS[po][:, 0:64], func=AF.Identity, scale=s4[:, 5:6])

                return [s0, s1, s2, s3, s4_, s5]

            it = 0
            for n in range(NT):
                ob, obb = osw[n % 2], OSB[n % 2]
                for j in range(0, 8, 2):
                    grp = [swa_iter(n, j, 0, it), swa_iter(n, j, 1, it + 1), swa_iter(n, j + 1, 0, it + 2), swa_iter(n, j + 1, 1, it + 3)]
                    it += 4
                    for fs in zip(*grp):
                        for f_ in fs:
                            f_()
                s4, s4b = stt2, STT2B
                S.op("act", "activation", (obb,), (J2B, s4b), out=junk2[:], in_=ob[:], func=AF.Square, accum_out=s4[:, 0:1])
                S.op("dve", "tensor_scalar", (s4b,), (s4b,), out=s4[:, 1:2], in0=s4[:, 0:1], scalar1=1.0 / 1024, scalar2=EPS, op0=ALU.mult, op1=ALU.add)
                S.op("act", "activation", (s4b,), (s4b,), out=s4[:, 2:3], in_=s4[:, 1:2], func=AF.Sqrt)
                S.op("dve", "reciprocal", (s4b,), (s4b,), out=s4[:, 3:4], in_=s4[:, 2:3])
                yb_, ybb = ysw[n % 2], YSB[n % 2]
                S.op("dve", "scalar_tensor_tensor", (obb, s4b, PRB), (ybb,), out=yb_[:], in0=ob[:], scalar=s4[:, 3:4], in1=anw[:], op0=ALU.mult, op1=ALU.mult)
                S.dma("sp", (ybb,), (), out=mix_d[n * 128:(n + 1) * 128, 1024:2048], in_=yb_[:])

        if STOP not in ("ln", "hgrn"):
            release(0)
            if STOP == "swa":
                dcs = [arena[:, 0:1024].bitcast(BF16), arena[:, 1024:2048].bitcast(BF16)]
                dfs = [arena[:, 2048:4096], arena[:, 4096:6144]]
                dB = [Buf(), Buf(), Buf(), Buf()]
                for i in range(NT):
                    S.dma("sp", (), (dB[i % 2],), out=dcs[i % 2], in_=mix_d[i * 128:(i + 1) * 128, :])
                    S.op("act", "copy", (dB[i % 2],), (dB[2 + i % 2],), out=dfs[i % 2], in_=dcs[i % 2])
                    out_tk.append(S.dma("sp", (dB[2 + i % 2],), (), out=dbg[i * 128:(i + 1) * 128, :], in_=dfs[i % 2]))
        if STOP in ("full", "moe", "h1"):
            g4 = al("g4", [128, NT, 4], F32)
            rowk = sb("rowk", [128, NT * 4], I32)
            gdT = al("gdT", [128, NT, 128], F32)
            masks = al("masks", [128, NT, E], BF16)
            G4B, RKB, GDTB, MKB = Buf(), Buf(), Buf(), Buf()
            mark_p = atop[0]
            wo = al("wo", [128, KD, D], BF16)
            WOB = [Buf() for _ in range(4)]
            for g in range(4):
                S.dma("pool", (WOUTB,), (WOB[g],), out=wo[:, g * 4:(g + 1) * 4, :], in_=w_out[g * 512:(g + 1) * 512, :].rearrange("(k p) c -> p k c", p=128))
            bo = al("bo", [128, D], F32)
            BOB = Buf()
            load_bcast(bo, BOB, b_out[0:1, :], D)
            load_bcast(pw, PWB, ln1_w[0:1, :], D)
            load_bcast(pb, PBB, ln1_b[0:1, :], D)
            wr = al("wr", [128, KD, E], F32)
            brt = al("brt", [128, E], F32)
            WRB = Buf()
            S.dma("sp", (), (WRB,), out=wr[:, :, :], in_=w_router.rearrange("(k p) e -> p k e", p=128))
            S.dma("sp", (), (WRB,), out=brt[:, :], in_=b_router[0:1, :].partition_broadcast(128))
            padt = al("padt", [128, E * C // 128 * 16], I32)
            PDB = Buf()
            S.op("pool", "memset", (), (PDB,), ap=padt[:], constant=SEQ)
            SLOTB = Buf()
            S.dma("sp", (PDB,), (SLOTB,), out=slot_d[0:E * C, :].rearrange("(p n) o -> p (n o)", p=128), in_=padt[:])
            S.dma("sp", (CB,), (), out=h1b_d[SEQ:SEQ + 128, :], in_=zer_b[:])
            mt = [al(f"mt{i}", [128, D], BF16) for i in range(2)]
            mT = [al(f"mT{i}", [128, KD, 128], BF16) for i in range(2)]
            u_ = [al(f"u{i}", [128, D], F32) for i in range(2)]
            h0t = [al("h0t0", [128, D], F32)] * 2
            h1t = [al(f"h1t{i}", [128, D], F32) for i in range(2)]
            h1bt = [al(f"h1bt{i}", [128, D], BF16) for i in range(2)]
            h1T = al("h1T", [128, KD, 128], F32)
            MTB, MTTB, UB, H0TB, H1TB, H1BB = ([Buf(), Buf()] for _ in range(6))
            H0TB = [H0TB[0]] * 2
            H1TTB = Buf()
            lg = al("lg", [128, E], F32)
            v8 = al("v8", [128, 8], F32)
            i8 = al("i8", [128, 8], U32)
            i8f = al("i8f", [128, 8], F32)
            rs = al("rs", [128, 8], F32)
            e4 = al("e4", [128, 4], F32)
            rowf = al("rowf", [128, E], F32)
            ovf = al("ovf", [128, E], F32)
            oh = al("oh", [128, E], F32)
            jE = al("jE", [128, E], F32)
            rk = al("rk", [128, 4], F32)
            gd = al("gd", [128, 128], F32)
            RTB = Buf()
            def front(i):
                p2 = i % 2
                rows = slice(i * 128, (i + 1) * 128)
                S.dma("sp", (), (MTB[p2],), out=mt[p2][:], in_=mix_d[rows, :])
                S.dma("sp", (), (H0TB[p2],), out=h0t[p2][:], in_=h0_d[rows, :])
                for g in range(4):
                    (pi,) = ps_get()
                    pv = PS[pi][:].bitcast(BF16)
                    for j in range(4):
                        kd = g * 4 + j
                        S.op("pe", "transpose", (MTB[p2],), (PSB[pi],), out=pv[:, j * 128:(j + 1) * 128], in_=mt[p2][:, kd * 128:(kd + 1) * 128], identity=ident_b[:])
                    S.op("dve" if g % 2 == 0 else "act", "tensor_copy" if g % 2 == 0 else "copy", (PSB[pi],), (MTTB[p2],),
                         out=mT[p2][:, g * 4:(g + 1) * 4, :], in_=pv[:, 0:512].rearrange("p (j t) -> p j t", j=4))
                banks = ps_get(4)
                for nb in range(4):
                    bk_ = banks[nb]
                    for kd in range(KD):
                        S.op("pe", "matmul", (MTTB[p2], WOB[kd // 4]), (PSB[bk_],), out=PS[bk_][:], lhsT=mT[p2][:, kd, :], rhs=wo[:, kd, nb * 512:(nb + 1) * 512], start=(kd == 0), stop=(kd == KD - 1))
                    cs = slice(nb * 512, (nb + 1) * 512)
                    S.op("dve", "tensor_tensor", (PSB[bk_], BOB), (UB[p2],), out=u_[p2][:, cs], in0=PS[bk_][:], in1=bo[:, cs], op=ALU.add)
                S.op("dve", "scalar_tensor_tensor", (H0TB[p2], UB[p2]), (UB[p2],), out=u_[p2][:], in0=h0t[p2][:], scalar=ALPHA, in1=u_[p2][:], op0=ALU.mult, op1=ALU.add)

            def back(i):
                p2 = i % 2
                rows = slice(i * 128, (i + 1) * 128)
                layer_norm(u_[p2], UB[p2], h1t[p2], H1TB[p2], pw, PWB, pb, PBB)
                S.dma("sp", (H1TB[p2],), (), out=h1_d[rows, :], in_=h1t[p2][:])
                S.op("act", "copy", (H1TB[p2],), (H1BB[p2],), out=h1bt[p2][:], in_=h1t[p2][:])
                S.dma("sp", (H1BB[p2],), (), out=h1b_d[rows, :], in_=h1bt[p2][:])
                for g in range(4):
                    (pi,) = ps_get()
                    for j in range(4):
                        kd = g * 4 + j
                        S.op("pe", "transpose", (H1TB[p2],), (PSB[pi],), out=PS[pi][:, j * 128:(j + 1) * 128], in_=h1t[p2][:, kd * 128:(kd + 1) * 128], identity=ident_f[:])
                    S.op("dve" if g % 2 == 0 else "act", "tensor_copy" if g % 2 == 0 else "copy", (PSB[pi],), (H1TTB,),
                         out=h1T[:, g * 4:(g + 1) * 4, :], in_=PS[pi][:, 0:512].rearrange("p (j t) -> p j t", j=4))
                (pl,) = ps_get()
                for kd in range(KD):
                    S.op("pe", "matmul", (H1TTB, WRB), (PSB[pl],), out=PS[pl][:, 0:E], lhsT=h1T[:, kd, :], rhs=wr[:, kd, :], start=(kd == 0), stop=(kd == KD - 1))
                R_ = (RTB,)
                S.op("dve", "tensor_tensor", (PSB[pl], WRB), R_, out=lg[:], in0=PS[pl][:, 0:E], in1=brt[:], op=ALU.add)
                S.op("dve", "max", R_, R_, out=v8[:], in_=lg[:])
                S.op("dve", "max_index", R_, R_, out=i8[:], in_max=v8[:], in_values=lg[:])
                S.op("dve", "tensor_scalar", R_, R_, out=rs[:, 0:1], in0=v8[:, 0:1], scalar1=-1.0, scalar2=None, op0=ALU.mult)
                S.op("act", "activation", R_, R_, out=e4[:], in_=v8[:, 0:4], func=AF.Exp, bias=rs[:, 0:1], scale=1.0, accum_out=rs[:, 1:2])
                S.op("dve", "reciprocal", R_, R_, out=rs[:, 2:3], in_=rs[:, 1:2])
                S.op("dve", "tensor_scalar", R_, R_ + (G4B,), out=g4[:, i, :], in0=e4[:], scalar1=rs[:, 2:3], scalar2=None, op0=ALU.mult)
                S.op("dve", "tensor_copy", R_, R_, out=i8f[:], in_=i8[:])
                S.op("dve", "tensor_scalar", R_, (MKB,), out=masks[:, i, :], in0=lg[:], scalar1=v8[:, 3:4], scalar2=None, op0=ALU.is_ge)
                (pp,) = ps_get()
                for j in range(i + 1):
                    S.op("pe", "matmul", (MKB, CB), (PSB[pp],), out=PS[pp][:, 0:E], lhsT=(lstrict if j == i else ones_b)[:], rhs=masks[:, j, :], start=(j == 0), stop=(j == i))
                S.op("dve", "tensor_tensor", (PSB[pp], CB), R_, out=rowf[:], in0=PS[pp][:, 0:E], in1=rowbase[:], op=ALU.add)
                S.op("dve", "tensor_scalar", (PSB[pp],), R_, out=ovf[:], in0=PS[pp][:, 0:E], scalar1=C - 0.5, scalar2=1.0, op0=ALU.is_ge, op1=ALU.mult)
                S.op("dve", "tensor_scalar", R_, R_, out=jE[:], in0=rowf[:], scalar1=-1.0, scalar2=float(E * C), op0=ALU.mult, op1=ALU.add)
                S.op("dve", "tensor_tensor", R_, R_, out=jE[:], in0=jE[:], in1=ovf[:], op=ALU.mult)
                S.op("dve", "tensor_tensor", R_, R_, out=rowf[:], in0=rowf[:], in1=jE[:], op=ALU.add)
                S.op("pool", "memset", (GDTB,), R_, ap=gd[:], constant=0.0)
                for k in range(4):
                    S.op("dve", "tensor_scalar", R_ + (CB,), R_, out=oh[:], in0=iota_e[:], scalar1=i8f[:, k:k + 1], scalar2=None, op0=ALU.is_equal)
                    S.op("dve", "tensor_tensor", R_, R_, out=jE[:], in0=oh[:], in1=rowf[:], op=ALU.mult)
                    S.op("dve", "reduce_sum", R_, R_, out=rk[:, k:k + 1], in_=jE[:], axis=AX.X)
                    S.op("dve", "scalar_tensor_tensor", R_ + (G4B,), R_, out=gd[:, 0:E], in0=oh[:], scalar=g4[:, i, k:k + 1], in1=gd[:, 0:E], op0=ALU.mult, op1=ALU.add)
                S.op("dve", "tensor_copy", R_, (RKB,), out=rowk[:, i * 4:(i + 1) * 4], in_=rk[:])
                (pg,) = ps_get()
                S.op("pe", "transpose", R_, (PSB[pg],), out=PS[pg][:, 0:128], in_=gd[:], identity=ident_f[:])
                S.op("act", "copy", (PSB[pg],), (GDTB,), out=gdT[:, i, :], in_=PS[pg][:, 0:128])
                for k in range(4):
                    S.dma("pool", (RKB, CB), (SLOTB,), meth="indirect_dma_start", out=slot_d[:, :], out_offset=IOA(ap=rowk[:, i * 4 + k:i * 4 + k + 1], axis=0),
                          in_=tokid[:, i * 16:(i + 1) * 16], in_offset=None)
            front(0)
            for i in range(NT):
                if i + 1 < NT:
                    front(i + 1)
                back(i)
            release(mark_p)
            if STOP == "h1":
                for i in range(NT):
                    out_tk.append(S.dma("sp", (), (), out=dbg[i * 128:(i + 1) * 128, :], in_=h1_d[i * 128:(i + 1) * 128, :]))

        if STOP in ("full", "moe"):
            FS = min(512, DFF)
            NS = DFF // FS
            FC = FS // 128
            NBLK = (E * KF + 127) // 128
            bgT = al("bgT", [128, NBLK * 128], F32)
            buT = al("buT", [128, NBLK * 128], F32)
            BGB = Buf()
            btmp = al("btmp", [128, 128], F32)
            BTB = Buf()
            for src, dst in ((b_gate, bgT), (b_up, buT)):
                for blk in range(NBLK):
                    nr = min(128, E * KF - blk * 128)
                    S.op("pool", "memset", (), (BTB,), ap=btmp[:], constant=0.0)
                    S.dma("sp", (), (BTB,), out=btmp[0:nr, :], in_=src[blk * 128: blk * 128 + nr, :])
                    (pi,) = ps_get()
                    S.op("pe", "transpose", (BTB,), (PSB[pi],), out=PS[pi][:, 0:128], in_=btmp[:], identity=ident_f[:])
                    S.op("act", "copy", (PSB[pi],), (BGB,), out=dst[:, blk * 128:(blk + 1) * 128], in_=PS[pi][:, 0:128])
            sidx = [sb(f"sidx{i}", [128, CT], I32) for i in range(2)]
            SIB = [Buf(), Buf()]
            xg = [al(f"xg{i}", [128, D], BF16) for i in range(2)]
            XGB = [Buf(), Buf()]
            xT = al("xT", [128, KD, C], BF16)
            XTB = Buf()
            actT = al("actT", [128, KF, C], BF16)
            ACB = Buf()
            NSL = cfg.get("NSL", 5)
            slabs = [al(f"slab{i}", [128, KD * 512], BF16) for i in range(NSL)]
            SLB = [Buf() for _ in range(NSL)]
            sln = [0]
            ysb = [al(f"ysb{i}", [128, 512], F32) for i in range(3)]
            YB = [Buf() for _ in range(3)]
            yn = [0]
            gt_ = [al(f"gt{i}", [128, 512], F32) for i in range(2)]
            ut_ = [al(f"ut{i}", [128, 512], F32) for i in range(2)]
            st_ = [al(f"st{i}", [128, 512], F32) for i in range(2)]
            GTB, UTB, STB2 = [Buf(), Buf()], [Buf(), Buf()], [Buf(), Buf()]
            en = [0]
            ccs = [(c0, min(512, C - c0)) for c0 in range(0, C, 512)]
            xT2 = al("xT2", [128, KD, C], BF16)
            XTB2 = Buf()
            xTs, XTBs = [xT, xT2], [XTB, XTB2]

            def prep(e):
                xT, XTB = xTs[e % 2], XTBs[e % 2]
                si, sib = sidx[e % 2], SIB[e % 2]
                S.dma("sp", (SLOTB,), (sib,), out=si[:, :], in_=slot_d[e * C:(e + 1) * C, 0:1].rearrange("(t p) o -> p (t o)", p=128), allow_slow_non_contiguous=True)
                for t in range(CT):
                    g_, gb_ = xg[t % 2], XGB[t % 2]
                    S.dma("pool", (sib,), (gb_,), meth="indirect_dma_start", out=g_[:], out_offset=None, in_=h1b_d[:, :], in_offset=IOA(ap=si[:, t:t + 1], axis=0))
                    for g in range(4):
                        (pi,) = ps_get()
                        pv = PS[pi][:].bitcast(BF16)
                        for j in range(4):
                            kd = g * 4 + j
                            S.op("pe", "transpose", (gb_,), (PSB[pi],), out=pv[:, j * 128:(j + 1) * 128], in_=g_[:, kd * 128:(kd + 1) * 128], identity=ident_b[:])
                        S.op("dve" if g % 2 == 0 else "act", "tensor_copy" if g % 2 == 0 else "copy", (PSB[pi],), (XTB,),
                             out=xT[:, g * 4:(g + 1) * 4, t * 128:(t + 1) * 128], in_=pv[:, 0:512].rearrange("p (j t) -> p j t", j=4))
            def gateup(e):
                xT, XTB = xTs[e % 2], XTBs[e % 2]
                for s in range(NS):
                    sg, sgb = slabs[sln[0] % NSL], SLB[sln[0] % NSL]
                    su, sub = slabs[(sln[0] + 1) % NSL], SLB[(sln[0] + 1) % NSL]
                    sln[0] += 2
                    sgv = sg[:, 0:KD * FS].rearrange("p (k f) -> p k f", k=KD)
                    suv = su[:, 0:KD * FS].rearrange("p (k f) -> p k f", k=KD)
                    ep_, el_ = e // EP, e % EP
                    S.dma("pool", (), (sgb,), out=sgv, in_=w_gate[ep_][el_ * D:(el_ + 1) * D, s * FS:(s + 1) * FS].rearrange("(k p) f -> p k f", p=128))
                    S.dma("pool", (), (sub,), out=suv, in_=w_up[ep_][el_ * D:(el_ + 1) * D, s * FS:(s + 1) * FS].rearrange("(k p) f -> p k f", p=128))
                    for fc in range(FC):
                        kf = s * FC + fc
                        bcol = e * KF + kf
                        for c0, cw in ccs:
                            pg_, pu_ = ps_get(2)
                            for kd in range(KD):
                                S.op("pe", "matmul", (sgb, XTB), (PSB[pg_],), out=PS[pg_][:, 0:cw], lhsT=sgv[:, kd, fc * 128:(fc + 1) * 128], rhs=xT[:, kd, c0:c0 + cw], start=(kd == 0), stop=(kd == KD - 1))
                            for kd in range(KD):
                                S.op("pe", "matmul", (sub, XTB), (PSB[pu_],), out=PS[pu_][:, 0:cw], lhsT=suv[:, kd, fc * 128:(fc + 1) * 128], rhs=xT[:, kd, c0:c0 + cw], start=(kd == 0), stop=(kd == KD - 1))
                            q2 = en[0] % 2
                            en[0] += 1
                            S.op("dve", "tensor_scalar", (PSB[pg_], BGB), (GTB[q2],), out=gt_[q2][:, 0:cw], in0=PS[pg_][:, 0:cw], scalar1=bgT[:, bcol:bcol + 1], scalar2=7.0, op0=ALU.add, op1=ALU.min)
                            S.op("act", "activation", (GTB[q2],), (STB2[q2],), out=st_[q2][:, 0:cw], in_=gt_[q2][:, 0:cw], func=AF.Sigmoid, scale=1.702)
                            S.op("dve", "tensor_scalar", (PSB[pu_], BGB), (UTB[q2],), out=ut_[q2][:, 0:cw], in0=PS[pu_][:, 0:cw], scalar1=buT[:, bcol:bcol + 1], scalar2=7.0, op0=ALU.add, op1=ALU.min)
                            S.op("dve", "tensor_scalar", (UTB[q2],), (UTB[q2],), out=ut_[q2][:, 0:cw], in0=ut_[q2][:, 0:cw], scalar1=-7.0, scalar2=1.0, op0=ALU.max, op1=ALU.add)
                            S.op("dve", "tensor_tensor", (GTB[q2], STB2[q2]), (GTB[q2],), out=gt_[q2][:, 0:cw], in0=gt_[q2][:, 0:cw], in1=st_[q2][:, 0:cw], op=ALU.mult)
                            S.op("dve", "tensor_tensor", (GTB[q2], UTB[q2]), (ACB,), out=actT[:, kf, c0:c0 + cw], in0=gt_[q2][:, 0:cw], in1=ut_[q2][:, 0:cw], op=ALU.mult)
            def down(e):
                for dn in range(4):
                    sd, sdb = slabs[sln[0] % NSL], SLB[sln[0] % NSL]
                    sln[0] += 1
                    sdv = sd[:, 0:KF * 512].rearrange("p (k m) -> p k m", k=KF)
                    ep_, el_ = e // EP, e % EP
                    S.dma("pool", (), (sdb,), out=sdv, in_=w_down[ep_][el_ * DFF:(el_ + 1) * DFF, dn * 512:(dn + 1) * 512].rearrange("(k p) m -> p k m", p=128))
                    for t in range(CT):
                        (py,) = ps_get()
                        for kf in range(KF):
                            S.op("pe", "matmul", (ACB, sdb), (PSB[py],), out=PS[py][:], lhsT=actT[:, kf, t * 128:(t + 1) * 128], rhs=sdv[:, kf, :], start=(kf == 0), stop=(kf == KF - 1))
                        y_, yb2 = ysb[yn[0] % 3], YB[yn[0] % 3]
                        yn[0] += 1
                        S.op("act" if yn[0] % 2 else "dve", "copy" if yn[0] % 2 else "tensor_copy", (PSB[py],), (yb2,), out=y_[:], in_=PS[py][:])
                        S.dma("sp", (yb2,), (), out=yall_d[e * C + t * 128: e * C + (t + 1) * 128, dn * 512:(dn + 1) * 512], in_=y_[:])
            prep(0)
            for e in range(E):
                gateup(e)
                if e + 1 < E:
                    prep(e + 1)
                down(e)
            release(mark_p)

            bd = al("bd", [128, D], F32)
            BDB = Buf()
            S.op("pool", "memset", (), (BDB,), ap=bd[:], constant=0.0)
            S.dma("sp", (), (BDB,), out=bd[0:E, :], in_=b_down[:, :])
            load_bcast(pw, PWB, ln2_w[0:1, :], D)
            load_bcast(pb, PBB, ln2_b[0:1, :], D)
            yk8 = [al(f"yk{i}", [128, D], F32) for i in range(8)]
            YKB8 = [Buf() for _ in range(8)]
            acc = [al(f"acc{i}", [128, D], F32) for i in range(2)]
            ACCB = [Buf(), Buf()]
            h1r = [al(f"h1r{i}", [128, D], F32) for i in range(2)]
            H1RB = [Buf(), Buf()]
            ot = [al(f"ot{i}", [128, D], F32) for i in range(2)]
            OTB = [Buf(), Buf()]
            for i in range(NT):
                p2 = i % 2
                rows = slice(i * 128, (i + 1) * 128)
                yk = yk8[p2 * 4:(p2 + 1) * 4]
                YKB = YKB8[p2 * 4:(p2 + 1) * 4]
                for k in range(4):
                    S.dma("pool", (RKB,), (YKB[k],), meth="indirect_dma_start", out=yk[k][:], out_offset=None, in_=yall_d[:, :],
                          in_offset=IOA(ap=rowk[:, i * 4 + k:i * 4 + k + 1], axis=0))
                S.dma("sp", (), (H1RB[p2],), out=h1r[p2][:], in_=h1_d[rows, :])
                banks = ps_get(4)
                for nb in range(4):
                    cs = slice(nb * 512, (nb + 1) * 512)
                    S.op("pe", "matmul", (GDTB, BDB), (PSB[banks[nb]],), out=PS[banks[nb]][:], lhsT=gdT[:, i, :], rhs=bd[:, cs], start=True, stop=True)
                    S.op("dve", "scalar_tensor_tensor", (YKB[0], G4B, PSB[banks[nb]]), (ACCB[p2],), out=acc[p2][:, cs], in0=yk[0][:, cs], scalar=g4[:, i, 0:1], in1=PS[banks[nb]][:], op0=ALU.mult, op1=ALU.add)
                for k in range(1, 4):
                    S.op("dve", "scalar_tensor_tensor", (YKB[k], G4B, ACCB[p2]), (ACCB[p2],), out=acc[p2][:], in0=yk[k][:], scalar=g4[:, i, k:k + 1], in1=acc[p2][:], op0=ALU.mult, op1=ALU.add)
                S.op("dve", "scalar_tensor_tensor", (H1RB[p2], ACCB[p2]), (ACCB[p2],), out=acc[p2][:], in0=h1r[p2][:], scalar=ALPHA, in1=acc[p2][:], op0=ALU.mult, op1=ALU.add)
                layer_norm(acc[p2], ACCB[p2], ot[p2], OTB[p2], pw, PWB, pb, PBB)
                out_tk.append(S.dma("sp", (OTB[p2],), (), out=(out if STOP == "full" else dbg)[rows, :], in_=ot[p2][:]))

        for q in ("sp",):
            S.wait_all(q, out_tk)
        block = st.enter_context(nc.Block())
        S.emit(block)
    return nc


FULL_CFG = dict(NCORES=8, E=32, ELOC=32, DFF=2048, C=384, SEQ=2048, NP=4)


def make_in_maps(cfg, inputs):
    NCORES, E, ELOC, DFF = cfg["NCORES"], cfg["E"], cfg["ELOC"], cfg["DFF"]
    f = lambda a: np.ascontiguousarray(np.asarray(a, dtype=np.float32))
    maps = []
    for c in range(NCORES):
        m = {
            "x": f(inputs["x"][c]),
            "ln_emb_w": f(inputs["ln_emb_w"]).reshape(1, D), "ln_emb_b": f(inputs["ln_emb_b"]).reshape(1, D),
            "w_in": f(inputs["w_in"][0]), "b_qkv": f(inputs["b_qkv"]).reshape(1, 1280),
            "lb_logits": f(inputs["lb_logits"]), "hg_norm_w": f(inputs["hg_norm_w"]).reshape(1, 128),
            "attn_sinks": f(inputs["attn_sinks"]).reshape(1, 16), "attn_norm_w": f(inputs["attn_norm_w"]).reshape(1, 1024),
            "w_out": f(inputs["w_out"][0]), "b_out": f(inputs["b_out"]).reshape(1, D),
            "ln1_w": f(inputs["ln1_w"]).reshape(1, D), "ln1_b": f(inputs["ln1_b"]).reshape(1, D),
            "w_router": f(inputs["w_router"][0]), "b_router": f(inputs["b_router"]).reshape(1, E),
            "b_gate": f(inputs["b_gate"][0]).reshape(E * DFF // 128, 128), "b_up": f(inputs["b_up"][0]).reshape(E * DFF // 128, 128),
            "b_down": f(inputs["b_down"][0]).reshape(E, D),
            "ln2_w": f(inputs["ln2_w"]).reshape(1, D), "ln2_b": f(inputs["ln2_b"]).reshape(1, D),
        }
        NP = cfg.get("NP", 1)
        EP = E // NP
        for p in range(NP):
            m[f"w_gate{p}"] = f(inputs["w_gate"][0][p * EP:(p + 1) * EP]).reshape(EP * D, DFF)
            m[f"w_up{p}"] = f(inputs["w_up"][0][p * EP:(p + 1) * EP]).reshape(EP * D, DFF)
            m[f"w_down{p}"] = f(inputs["w_down"][0][p * EP:(p + 1) * EP]).reshape(EP * DFF, D)
        maps.append(m)
    return maps


def kernel(**inputs):
    cfg = FULL_CFG
    nc = build(cfg)
    maps = make_in_maps(cfg, inputs)
    res = run_bass_kernel_spmd(nc, maps, core_ids=list(range(cfg["NCORES"])))
    return np.stack([np.asarray(r["out"], dtype=np.float32) for r in res.results], axis=0)
```
